# Optimizing a Trainium2 kernel written in Bass

```python
import jax, jax.numpy as jnp
from jax import lax
import numpy as np

D_MODEL = 4096
BATCH = 16
SEQ = 256
DEPTH = 1
DEC_BATCH = 8
DEC_SEQ = 4096
PAST_LEN = 512

GRID_W = 64
EPS = 1e-6
ROPE_BASE = 10000.0
Q_BLOCK = 128
N_MOD = 6
MLA_HEADS = 16
MLA_Q_RANK = 1024
MLA_KV_RANK = 512
MLA_NOPE = 128
MLA_ROPE = 64
MLA_V = 128
GQA_HEADS = 16
GQA_KV_HEADS = 4
HEAD_DIM = 128
PEER_HEADS = 8
N_KEYS = 128
N_EXPERTS = N_KEYS * N_KEYS
PEER_QDIM = 256
PEER_TOPK = 16
PEER_CHUNK = 128
IN_SIZES = (MLA_Q_RANK, MLA_KV_RANK, MLA_ROPE, GQA_HEADS * HEAD_DIM, GQA_KV_HEADS * HEAD_DIM,
            GQA_KV_HEADS * HEAD_DIM, D_MODEL, D_MODEL)
IN_COLS = MLA_Q_RANK + MLA_KV_RANK + MLA_ROPE + GQA_HEADS * HEAD_DIM + 2 * GQA_KV_HEADS * HEAD_DIM + 2 * D_MODEL

kernel_name = "hybrid_mla_gqa_peer_diffusion_step"


def rmsnorm(x, g):
    x32 = x.astype(jnp.float32)
    y = x32 * lax.rsqrt(jnp.mean(x32 * x32, axis=-1, keepdims=True) + EPS)
    return (y * g.astype(jnp.float32)).astype(x.dtype)


def axial_rope_tables(n_tokens, dim):
    n_rows = n_tokens // GRID_W
    rows = jnp.broadcast_to(jnp.arange(n_rows)[:, None], (n_rows, GRID_W)).reshape(-1).astype(jnp.float32)
    cols = jnp.broadcast_to(jnp.arange(GRID_W)[None, :], (n_rows, GRID_W)).reshape(-1).astype(jnp.float32)
    n_freq = dim // 4
    freqs = ROPE_BASE ** (-jnp.arange(n_freq, dtype=jnp.float32) / n_freq)
    ang = jnp.concatenate([rows[:, None] * freqs, cols[:, None] * freqs], axis=-1)
    return (jnp.cos(ang), jnp.sin(ang))


def apply_rope(x, cos, sin):
    shape = (cos.shape[0],) + (1,) * (x.ndim - 3) + (cos.shape[1],)
    cs = cos.reshape(shape)
    sn = sin.reshape(shape)
    xp = x.astype(jnp.float32).reshape(x.shape[:-1] + (x.shape[-1] // 2, 2))
    x1, x2 = xp[..., 0], xp[..., 1]
    out = jnp.stack([x1 * cs - x2 * sn, x1 * sn + x2 * cs], axis=-1).reshape(x.shape)
    return out.astype(x.dtype)


def blocked_attention(q, k, v, scale):
    b, t, h, dq = q.shape
    g = k.shape[2]
    rep = h // g
    dv = v.shape[-1]
    qb = q.reshape(b, t // Q_BLOCK, Q_BLOCK, g, rep, dq).transpose(1, 0, 2, 3, 4, 5)

    def one_block(q_blk):
        s = jnp.einsum('bqgrd,bsgd->bgrqs', q_blk, k).astype(jnp.float32) * scale
        p = jax.nn.softmax(s, axis=-1).astype(v.dtype)
        return jnp.einsum('bgrqs,bsgd->bqgrd', p, v)

    o = lax.map(one_block, qb)
    return o.transpose(1, 0, 2, 3, 4, 5).reshape(b, t, h, dv)


def token_mixer(h, lw, rope, ctx):
    b, t, _ = h.shape
    proj = h @ lw['w_in']
    pts = []
    acc = 0
    for sz in IN_SIZES[:-1]:
        acc += sz
        pts.append(acc)
    q_a, kv_a, k_rope, gq, gk, gv, gate_mla, gate_gqa = jnp.split(proj, pts, axis=-1)
    q = (rmsnorm(q_a, lw['g_q_a']) @ lw['w_q_b']).reshape(b, t, MLA_HEADS, MLA_NOPE + MLA_ROPE)
    q_nope, q_rope = q[..., :MLA_NOPE], q[..., MLA_NOPE:]
    ckv = rmsnorm(kv_a, lw['g_kv_a'])
    gq = rmsnorm(gq.reshape(b, t, GQA_HEADS, HEAD_DIM), lw['g_gqa_q'])
    gk = rmsnorm(gk.reshape(b, t, GQA_KV_HEADS, HEAD_DIM), lw['g_gqa_k'])
    gv = gv.reshape(b, t, GQA_KV_HEADS, HEAD_DIM)
    if rope is not None:
        cos_m, sin_m, cos_g, sin_g = rope
        q_rope = apply_rope(q_rope, cos_m, sin_m)
        k_rope = apply_rope(k_rope, cos_m, sin_m)
        gq = apply_rope(gq, cos_g, sin_g)
        gk = apply_rope(gk, cos_g, sin_g)
    own = (ckv, k_rope, gk, gv)
    if ctx is not None:
        ckv_all = jnp.concatenate([ckv, ctx[0]], axis=1)
        kr_all = jnp.concatenate([k_rope, ctx[1]], axis=1)
        gk_all = jnp.concatenate([gk, ctx[2]], axis=1)
        gv_all = jnp.concatenate([gv, ctx[3]], axis=1)
    else:
        ckv_all, kr_all, gk_all, gv_all = own
    s_len = ckv_all.shape[1]
    kv = (ckv_all @ lw['w_kv_b']).reshape(b, s_len, MLA_HEADS, MLA_NOPE + MLA_V)
    k_mla = jnp.concatenate(
        [kv[..., :MLA_NOPE], jnp.broadcast_to(kr_all[:, :, None, :], (b, s_len, MLA_HEADS, MLA_ROPE))], axis=-1)
    v_mla = kv[..., MLA_NOPE:]
    q_mla = jnp.concatenate([q_nope, q_rope], axis=-1)
    o_mla = blocked_attention(q_mla, k_mla, v_mla, (MLA_NOPE + MLA_ROPE) ** -0.5).reshape(b, t, MLA_HEADS * MLA_V)
    o_gqa = blocked_attention(gq, gk_all, gv_all, HEAD_DIM ** -0.5).reshape(b, t, GQA_HEADS * HEAD_DIM)
    merged = (jax.nn.sigmoid(gate_mla) * (o_mla @ lw['w_mla_o'])
              + jax.nn.sigmoid(gate_gqa) * (o_gqa @ lw['w_gqa_o']))
    return merged @ lw['w_out'], own


def peer(h, lw):
    b, t, d = h.shape
    n = b * t
    xf = h.reshape(n, d)
    q = (xf @ lw['w_peer_q']).reshape(n, PEER_HEADS, 2, PEER_QDIM // 2)
    s = jnp.einsum('nhpk,pmk->nhpm', q, lw['peer_sub_keys']).astype(jnp.float32)
    sv, si = lax.top_k(s, PEER_TOPK)
    cand = sv[..., 0, :, None] + sv[..., 1, None, :]
    cand_idx = si[..., 0, :, None] * N_KEYS + si[..., 1, None, :]
    best, pos = lax.top_k(cand.reshape(n, PEER_HEADS, PEER_TOPK * PEER_TOPK), PEER_TOPK)
    experts = jnp.take_along_axis(cand_idx.reshape(n, PEER_HEADS, PEER_TOPK * PEER_TOPK), pos, axis=-1)
    gates = jax.nn.softmax(best, axis=-1).astype(h.dtype)
    n_chunks = n // PEER_CHUNK

    def one_chunk(args):
        x_c, e_c, g_c = args
        u = jnp.take(lw['peer_u'], e_c, axis=0)
        a = jnp.einsum('cd,ced->ce', x_c, u)
        w = jax.nn.gelu(a, approximate=False) * g_c
        vv = jnp.take(lw['peer_v'], e_c, axis=0)
        return jnp.einsum('ce,ced->cd', w, vv)

    out = lax.map(one_chunk, (xf.reshape(n_chunks, PEER_CHUNK, d),
                              experts.reshape(n_chunks, PEER_CHUNK, PEER_HEADS * PEER_TOPK),
                              gates.reshape(n_chunks, PEER_CHUNK, PEER_HEADS * PEER_TOPK)))
    return out.reshape(b, t, d)


def modulation(cvec, w_mod, b_mod):
    m = jax.nn.silu(cvec) @ w_mod + b_mod
    return jnp.split(m, N_MOD, axis=-1)


def layer(x, mods, lw, rope, ctx):
    shift1, scale1, gate1, shift2, scale2, gate2 = mods
    h = rmsnorm(x, lw['g_norm1']) * (1 + scale1) + shift1
    o, own = token_mixer(h, lw, rope, ctx)
    x = x + gate1 * o
    h2 = rmsnorm(x, lw['g_norm2']) * (1 + scale2) + shift2
    x = x + gate2 * peer(h2, lw)
    return x, own


def setup_inputs(seed: int = 0) -> dict:
    key = jax.random.key(seed)
    ks = jax.random.split(key, 32)
    f32 = jnp.float32

    def nrm(k, shape, scale):
        return jax.random.normal(k, shape, f32) * scale

    def gain(k, shape):
        return 1.0 + 0.01 * jax.random.normal(k, shape, f32)

    return {
        'x_prompt': nrm(ks[0], (BATCH, SEQ, D_MODEL), 1.0),
        'x_sample': nrm(ks[1], (DEC_BATCH, DEC_SEQ, D_MODEL), 1.0),
        'c': nrm(ks[2], (DEC_BATCH, D_MODEL), 1.0),
        'cache_mla_ckv': nrm(ks[3], (DEC_BATCH, DEPTH, PAST_LEN, MLA_KV_RANK), 1.0),
        'cache_mla_krope': nrm(ks[4], (DEC_BATCH, DEPTH, PAST_LEN, MLA_ROPE), 1.0),
        'cache_gqa_k': nrm(ks[5], (DEC_BATCH, DEPTH, PAST_LEN, GQA_KV_HEADS, HEAD_DIM), 1.0),
        'cache_gqa_v': nrm(ks[6], (DEC_BATCH, DEPTH, PAST_LEN, GQA_KV_HEADS, HEAD_DIM), 1.0),
        'c_ctx': nrm(ks[7], (D_MODEL,), 1.0),
        'w_mod': nrm(ks[8], (DEPTH, D_MODEL, N_MOD * D_MODEL), 0.5 * D_MODEL ** -0.5),
        'b_mod': nrm(ks[9], (DEPTH, N_MOD * D_MODEL), 0.01),
        'g_norm1': gain(ks[10], (DEPTH, D_MODEL)),
        'g_norm2': gain(ks[11], (DEPTH, D_MODEL)),
        'w_in': nrm(ks[12], (DEPTH, D_MODEL, IN_COLS), D_MODEL ** -0.5),
        'g_q_a': gain(ks[13], (DEPTH, MLA_Q_RANK)),
        'w_q_b': nrm(ks[14], (DEPTH, MLA_Q_RANK, MLA_HEADS * (MLA_NOPE + MLA_ROPE)), MLA_Q_RANK ** -0.5),
        'g_kv_a': gain(ks[15], (DEPTH, MLA_KV_RANK)),
        'w_kv_b': nrm(ks[16], (DEPTH, MLA_KV_RANK, MLA_HEADS * (MLA_NOPE + MLA_V)), MLA_KV_RANK ** -0.5),
        'g_gqa_q': gain(ks[17], (DEPTH, HEAD_DIM)),
        'g_gqa_k': gain(ks[18], (DEPTH, HEAD_DIM)),
        'w_mla_o': nrm(ks[19], (DEPTH, MLA_HEADS * MLA_V, D_MODEL), (MLA_HEADS * MLA_V) ** -0.5),
        'w_gqa_o': nrm(ks[20], (DEPTH, GQA_HEADS * HEAD_DIM, D_MODEL), (GQA_HEADS * HEAD_DIM) ** -0.5),
        'w_out': nrm(ks[21], (DEPTH, D_MODEL, D_MODEL), D_MODEL ** -0.5),
        'w_peer_q': nrm(ks[22], (DEPTH, D_MODEL, PEER_HEADS * PEER_QDIM), D_MODEL ** -0.5),
        'peer_sub_keys': nrm(ks[23], (DEPTH, 2, N_KEYS, PEER_QDIM // 2), (PEER_QDIM // 2) ** -0.5),
        'peer_u': nrm(ks[24], (DEPTH, N_EXPERTS, D_MODEL), D_MODEL ** -0.5),
        'peer_v': nrm(ks[25], (DEPTH, N_EXPERTS, D_MODEL), PEER_HEADS ** -0.5),
        'g_final': gain(ks[26], (D_MODEL,)),
    }


def reference(x_prompt, x_sample, c, cache_mla_ckv, cache_mla_krope, cache_gqa_k, cache_gqa_v,
              c_ctx, w_mod, b_mod, g_norm1, g_norm2, w_in, g_q_a, w_q_b, g_kv_a, w_kv_b,
              g_gqa_q, g_gqa_k, w_mla_o, w_gqa_o, w_out, w_peer_q, peer_sub_keys, peer_u, peer_v,
              g_final):
    t_lat = x_sample.shape[1]
    rope = axial_rope_tables(t_lat, MLA_ROPE) + axial_rope_tables(t_lat, HEAD_DIM)
    xc = x_prompt
    xl = x_sample
    new_ckv, new_kr, new_k, new_v = [], [], [], []
    for l in range(DEPTH):
        lw = {
            'g_norm1': g_norm1[l], 'g_norm2': g_norm2[l], 'w_in': w_in[l],
            'g_q_a': g_q_a[l], 'w_q_b': w_q_b[l], 'g_kv_a': g_kv_a[l], 'w_kv_b': w_kv_b[l],
            'g_gqa_q': g_gqa_q[l], 'g_gqa_k': g_gqa_k[l],
            'w_mla_o': w_mla_o[l], 'w_gqa_o': w_gqa_o[l], 'w_out': w_out[l],
            'w_peer_q': w_peer_q[l], 'peer_sub_keys': peer_sub_keys[l],
            'peer_u': peer_u[l], 'peer_v': peer_v[l],
        }
        mods_ctx = modulation(c_ctx, w_mod[l], b_mod[l])
        mods_lat = modulation(c[:, None, :], w_mod[l], b_mod[l])
        xc, own_ctx = layer(xc, mods_ctx, lw, None, None)
        new_ckv.append(own_ctx[0])
        new_kr.append(own_ctx[1])
        new_k.append(own_ctx[2])
        new_v.append(own_ctx[3])
        ctx = (cache_mla_ckv[:, l], cache_mla_krope[:, l], cache_gqa_k[:, l], cache_gqa_v[:, l])
        xl, _ = layer(xl, mods_lat, lw, rope, ctx)
    y_prompt = rmsnorm(xc, g_final)
    y_sample = rmsnorm(xl, g_final)
    return (y_prompt, y_sample, jnp.stack(new_ckv, axis=1), jnp.stack(new_kr, axis=1),
            jnp.stack(new_k, axis=1), jnp.stack(new_v, axis=1))
```

```python
import numpy as np
from contextlib import ExitStack
import concourse.bass as bass
import concourse.mybir as mybir
from concourse.bass_utils import run_bass_kernel_spmd

F32 = mybir.dt.float32
BF16 = mybir.dt.bfloat16
U32 = mybir.dt.uint32
AF = mybir.ActivationFunctionType
ALU = mybir.AluOpType
AX = mybir.AxisListType

D = 4096
EPS = 1e-6
NDS = 48
NEG = -1.0e30


class Buf:
    __slots__ = ("w", "r", "multi")

    def __init__(self, multi=False):
        self.w = {}
        self.r = {}
        self.multi = multi


class KB:
    def __init__(self, nc, es):
        self.nc = nc
        self.E = {"pe": nc.tensor, "act": nc.scalar, "dve": nc.vector, "pool": nc.gpsimd, "sp": nc.sync}
        self.sem = {e: es.enter_context(nc.semaphore("sem_" + e)) for e in ("pe", "act", "dve", "pool")}
        self.cnt = {e: 0 for e in self.sem}
        self.seen = {e: {} for e in self.E}
        self.dsems = [es.enter_context(nc.semaphore("dsem%d" % i)) for i in range(NDS)]
        self.dval = [0] * NDS
        self.dnext = {"sp": 0, "pool": 0, "act": 0}
        self.drange = {"sp": (0, 32), "pool": (32, NDS), "act": (0, 32)}

    def _wait(self, eng, key, val):
        seen = self.seen[eng]
        if seen.get(key, 0) >= val:
            return
        if key[0] == "e":
            if key[1] == "pe":
                assert val <= self.cnt["pe"], "wait on unmarked PE instruction"
            sem = self.sem[key[1]]
        else:
            sem = self.dsems[key[1]]
        self.E[eng].wait_ge(sem, val)
        seen[key] = val

    def _deps(self, eng, reads, writes):
        need = {}
        for b in reads:
            for kk, v in b.w.items():
                if need.get(kk, 0) < v:
                    need[kk] = v
        for b in writes:
            if not b.multi:
                for kk, v in b.w.items():
                    if need.get(kk, 0) < v:
                        need[kk] = v
            for kk, v in b.r.items():
                if need.get(kk, 0) < v:
                    need[kk] = v
        for kk, v in need.items():
            if eng == "pe" and kk == ("e", "pe"):
                continue
            self._wait(eng, kk, v)

    def _post(self, key, val, reads, writes):
        for b in reads:
            if b.r.get(key, 0) < val:
                b.r[key] = val
        for b in writes:
            if b.multi:
                if b.w.get(key, 0) < val:
                    b.w[key] = val
            else:
                b.w = {key: val}
                b.r = {}

    def op(self, eng, fn, reads=(), writes=(), mark=True):
        self._deps(eng, reads, writes)
        ins = fn()
        val = self.cnt[eng] + 1
        if mark:
            ins.then_inc(self.sem[eng], 1)
            self.cnt[eng] = val
        self._post(("e", eng), val, reads, writes)

    def dma(self, q, out, in_, reads=(), writes=(), **kw):
        self._deps(q, reads, writes)
        lo, hi = self.drange[q]
        slot = lo + self.dnext[q]
        self.dnext[q] = (self.dnext[q] + 1) % (hi - lo)
        if self.dval[slot] > 0:
            self._wait(q, ("d", slot), self.dval[slot])
        ins = self.E[q].dma_start(out=out, in_=in_, **kw)
        self.dval[slot] += 16
        ins.then_inc(self.dsems[slot], 16)
        self._post(("d", slot), self.dval[slot], reads, writes)

    def barrier(self):
        for eng in self.E:
            for e in self.sem:
                if self.cnt[e] > 0:
                    self._wait(eng, ("e", e), self.cnt[e])
            for s in range(NDS):
                if self.dval[s] > 0:
                    self._wait(eng, ("d", s), self.dval[s])


class Ring:
    def __init__(self, tiles, multi=False):
        self.tiles = tiles
        self.bufs = [Buf(multi) for _ in tiles]
        self.i = 0

    def next(self):
        t, b = self.tiles[self.i], self.bufs[self.i]
        self.i = (self.i + 1) % len(self.tiles)
        return t, b


def build(cfg):
    TS, TC, TP, NPB = cfg["TS"], cfg["TC"], cfg["TP"], cfg["NPB"]
    debug = cfg.get("debug", ())
    stop_after = cfg.get("stop_after", 9)
    NQ = TS + NPB * TP
    NK = TS + TC + NPB * TP
    TG = 512
    assert TS % TG == 0 and TC % TG == 0 and NPB * TP == TG
    NTG = NQ // TG
    NKG = NK // TG
    NKT = NK // 128
    NQT = NQ // 128
    seqs = [(0, TS, 0, TS + TC)]
    for i in range(NPB):
        seqs.append((TS + i * TP, TP, TS + TC + i * TP, TP))

    nc = bass.Bass("TRN2", target_bir_lowering=False)

    only = cfg.get("only")
    dins = []

    def din(name, shape, dt=F32):
        if only and name not in ("sub_keys",):
            shape = [2, 2]
        dins.append((name, list(shape)))
        return nc.dram_tensor(name, list(shape), dt, kind="ExternalInput").ap()

    def dout(name, shape, dt=F32):
        return nc.dram_tensor(name, list(shape), dt, kind="ExternalOutput").ap()

    def dscr(name, shape, dt=BF16):
        kind = "ExternalOutput" if name in debug else "Internal"
        return nc.dram_tensor(name, list(shape), dt, kind=kind).ap()

    xin = din("xin", [NQ, D])
    cvec = din("cvec", [2, D])
    c_ckv = din("c_ckv", [TC, 512])
    c_kr = din("c_kr", [TC, 64])
    c_gk = din("c_gk", [TC, 512])
    c_gv = din("c_gv", [TC, 512])
    w_mod = din("w_mod", [D, 6 * D])
    b_mod = din("b_mod", [6 * D])
    g_norm1 = din("g_norm1", [D])
    g_norm2 = din("g_norm2", [D])
    w_in = din("w_in", [D, 12864])
    g_q_a = din("g_q_a", [1024])
    w_q_b = din("w_q_b", [1024, 3072])
    g_kv_a = din("g_kv_a", [512])
    w_kv_b = din("w_kv_b", [512, 4096])
    g_gqa_q = din("g_gqa_q", [128])
    g_gqa_k = din("g_gqa_k", [128])
    w_mla_o = din("w_mla_o", [2048, D])
    w_gqa_o = din("w_gqa_o", [2048, D])
    w_out = din("w_out", [D, D])
    w_peer_q = din("w_peer_q", [D, 2048])
    sub_keys = din("sub_keys", [2, 128, 128])
    peer_u = din("peer_u", [16384, D])
    peer_v = din("peer_v", [16384, D])
    g_final = din("g_final", [D])
    rope_m = din("rope_m", [TS, 2, 32])
    rope_g = din("rope_g", [TS, 2, 64])

    y = dout("y", [NQ, D])
    n_ckv = dout("n_ckv", [NPB * TP, 512])
    n_kr = dout("n_kr", [NPB * TP, 64])
    n_gk = dout("n_gk", [NPB * TP, 512])
    n_gv = dout("n_gv", [NPB * TP, 512])

    w_in_b = dscr("w_in_b", [D, 12864])
    w_q_bb = dscr("w_q_bb", [1024, 3072])
    w_kv_bb = dscr("w_kv_bb", [512, 4096])
    w_mla_ob = dscr("w_mla_ob", [2048, D])
    w_gqa_ob = dscr("w_gqa_ob", [2048, D])
    w_out_b = dscr("w_out_b", [D, D])
    w_pq_b = dscr("w_pq_b", [D, 2048])
    pv_b = dscr("pv_b", [16384, D])
    ut_b = dscr("ut_b", [128, 128, 32, 128])
    mods = dscr("mods", [2, 6 * D], F32)
    qan = dscr("qan", [NQ, 1024])
    ckv_s = dscr("ckv_s", [NK, 512])
    qTn = dscr("qTn", [16, 128, NQ])
    qTr = dscr("qTr", [16, 64, NQ])
    kTn = dscr("kTn", [16, 128, NK])
    kTr = dscr("kTr", [64, NK])
    vm = dscr("vm", [16, 128, NKT, 128])
    gqT = dscr("gqT", [16, 128, NQ])
    gkT = dscr("gkT", [4, 128, NK])
    gvm = dscr("gvm", [4, 128, NKT, 128])
    o_mla = dscr("o_mla", [NQ, 2048])
    o_gqa = dscr("o_gqa", [NQ, 2048])
    merged = dscr("merged", [NQ, D])
    x1 = dscr("x1", [NQ, D], F32)
    Gs = dscr("Gs", [NQT, 128, 128, 128])
    if only == "d1b":
        qpT = nc.dram_tensor("qpT", [16, 128, NQ], BF16, kind="ExternalInput").ap()
    else:
        qpT = dscr("qpT", [16, 128, NQ])

    es = ExitStack()
    with es:
        k = KB(nc, es)

        def sb(st, name, shape, dt):
            return st.enter_context(nc.sbuf_tensor(name, list(shape), dt))

        pf = [es.enter_context(nc.psum_tensor("pf%d" % i, [128, 512], F32)) for i in range(6)]
        pfb = [Buf() for _ in range(6)]
        ptb = [es.enter_context(nc.psum_tensor("ptb%d" % i, [128, 1024], BF16)) for i in range(2)]
        pT = Ring(ptb)

        class PRing:
            def __init__(self, idx):
                self.idx = idx
                self.i = 0

            def next(self):
                j = self.idx[self.i]
                self.i = (self.i + 1) % len(self.idx)
                return pf[j], pfb[j]

        ident_f = sb(es, "ident_f", [128, 128], F32)
        ident = sb(es, "ident", [128, 128], BF16)
        iota_f = sb(es, "iota_f", [128, 128], F32)
        cb_ = Buf()
        k.op("pool", lambda: nc.gpsimd.iota(ident_f[:], pattern=[[1, 128]], base=0, channel_multiplier=-1,
                                            allow_small_or_imprecise_dtypes=True), writes=[cb_])
        k.op("pool", lambda: nc.gpsimd.iota(iota_f[:], pattern=[[1, 128]], base=0, channel_multiplier=0,
                                            allow_small_or_imprecise_dtypes=True), writes=[cb_])
        k.op("dve", lambda: nc.vector.tensor_single_scalar(out=ident[:], in_=ident_f[:], scalar=0.0, op=ALU.is_equal),
             reads=[cb_], writes=[cb_])
        k.op("dve", lambda: nc.vector.tensor_single_scalar(out=ident_f[:], in_=ident_f[:], scalar=0.0, op=ALU.is_equal),
             reads=[cb_], writes=[cb_])
        fm = sb(es, "fm", [128, 2, 4, 32], F32)
        gn = sb(es, "gn", [128, 2, 32], F32)
        Gm = sb(es, "Gm", [128, 2, 2, 32], F32)
        modb = Buf()

        wbuf0 = Buf(multi=True)

        wb_of = {}
        deferred = []

        def cast_copy(dst, src, rows, rchunk, c0=0, c1=None, defer=False):
            if only:
                return
            b = wb_of.setdefault(dst.name, Buf(multi=True))
            for r0 in range(0, rows, rchunk):
                if c1 is None:
                    o_, i_ = dst[r0:r0 + rchunk, :], src[r0:r0 + rchunk, :]
                else:
                    o_, i_ = dst[r0:r0 + rchunk, c0:c1], src[r0:r0 + rchunk, c0:c1]
                fn = (lambda o_=o_, i_=i_, b=b: k.dma("pool", out=o_, in_=i_, writes=[b], max_dma_last_dim=8192))
                if defer:
                    deferred.append(fn)
                else:
                    fn()

        def flush_casts(n):
            for _ in range(min(n, len(deferred))):
                deferred.pop(0)()

        if only:
            stop_after = -1
        cast_copy(w_in_b, w_in, D, 512, 0, 4672)
        cast_copy(w_q_bb, w_q_b, 1024, 512, defer=True)
        cast_copy(w_kv_bb, w_kv_b, 512, 512, defer=True)
        cast_copy(ckv_s[TS:TS + TC, :], c_ckv, TC, 512, defer=True)
        if stop_after > 3:
            cast_copy(w_mla_ob, w_mla_o, 2048, 512, defer=True)
            cast_copy(w_gqa_ob, w_gqa_o, 2048, 512, defer=True)
            cast_copy(w_in_b, w_in, D, 512, 4672, 12864, defer=True)
            cast_copy(w_out_b, w_out, D, 512, defer=True)
        if stop_after > 5:
            cast_copy(w_pq_b, w_peer_q, D, 1024, defer=True)
            cast_copy(pv_b, peer_v, 16384, 1024, defer=True)
        if stop_after >= 1:
            with ExitStack() as ps:
                cT = sb(ps, "cT", [128, 2, 32], F32)
                sc = sb(ps, "sc", [128, 2, 32], BF16)
                cTb = Buf()
                wm = Ring([sb(ps, "wm%d" % i, [128, 32, 512], BF16) for i in range(2)])
                bb = Ring([sb(ps, "bb%d" % i, [2, 512], F32) for i in range(2)])
                mst = Ring([sb(ps, "mst%d" % i, [2, 512], F32) for i in range(2)])
                mring = PRing([0, 1])
                Rv = sb(ps, "Rv", [32, 12, 128], F32)
                Rvb = Buf()
                k.dma("sp", out=Rv[:, 0:2, :], in_=cvec.rearrange("s (kc p) -> kc s p", p=128), writes=[Rvb])
                ptv, pbv = mring.next()
                for v in range(2):
                    k.op("pe", lambda: nc.tensor.transpose(ptv[:, v * 32:(v + 1) * 32], Rv[0:32, v, :], ident_f[0:32, 0:32]),
                         reads=[Rvb, cb_], writes=[pbv], mark=(v == 1))
                k.op("dve", lambda: nc.vector.tensor_copy(out=cT[:], in_=ptv[:, 0:64].rearrange("p (s k) -> p s k", k=32)),
                     reads=[pbv], writes=[cTb])
                k.op("act", lambda: nc.scalar.activation(out=sc[:], in_=cT[:], func=AF.Silu), reads=[cTb], writes=[cTb])
                wmv = w_mod.rearrange("(kc p) n -> p kc n", p=128)
                for c in range(48):
                    wt, wb = wm.next()
                    k.dma("pool", out=wt[:], in_=wmv[:, :, c * 512:(c + 1) * 512], writes=[wb])
                    bt, bbf = bb.next()
                    k.dma("sp", out=bt[:], in_=b_mod[c * 512:(c + 1) * 512].partition_broadcast(2), writes=[bbf])
                    pt, pb = mring.next()
                    for kc in range(32):
                        k.op("pe", lambda: nc.tensor.matmul(pt[0:2, :], lhsT=sc[:, :, kc], rhs=wt[:, kc, :],
                                                            start=(kc == 0), stop=(kc == 31)),
                             reads=[cTb, wb], writes=[pb], mark=(kc == 31))
                    mt, mb = mst.next()
                    k.op("dve", lambda: nc.vector.tensor_tensor(out=mt[:], in0=pt[0:2, :], in1=bt[:], op=ALU.add),
                         reads=[pb, bbf], writes=[mb])
                    k.dma("sp", out=mods[:, c * 512:(c + 1) * 512], in_=mt[:], reads=[mb], writes=[modb])
                mv = mods.rearrange("s (v kc p) -> kc s v p", p=128, kc=32)
                vi = 0
                for s in range(2):
                    for a, v in enumerate((1, 0, 4, 3)):
                        k.dma("sp", out=Rv[:, vi, :], in_=mv[:, s, v, :], reads=[modb, Rvb], writes=[Rvb])
                        vi += 1
                k.dma("sp", out=Rv[:, 8, :], in_=g_norm1.rearrange("(kc p) -> kc p", p=128), writes=[Rvb])
                k.dma("sp", out=Rv[:, 9, :], in_=g_norm2.rearrange("(kc p) -> kc p", p=128), writes=[Rvb])
                ptv, pbv = mring.next()
                for v in range(10):
                    k.op("pe", lambda: nc.tensor.transpose(ptv[:, v * 32:(v + 1) * 32], Rv[0:32, v, :], ident_f[0:32, 0:32]),
                         reads=[Rvb, cb_], writes=[pbv], mark=(v == 9))
                k.op("dve", lambda: nc.vector.tensor_copy(out=fm[:].rearrange("p s a k -> p (s a k)"), in_=ptv[:, 0:256]),
                     reads=[pbv], writes=[cb_])
                k.op("dve", lambda: nc.vector.tensor_copy(out=gn[:].rearrange("p a k -> p (a k)"), in_=ptv[:, 256:320]),
                     reads=[pbv], writes=[cb_])
                for s in range(2):
                    for a in range(2):
                        k.op("dve", lambda: nc.vector.scalar_tensor_tensor(
                            out=Gm[:, s, a, :], in0=fm[:, s, 2 * a, :], scalar=1.0, in1=gn[:, a, :],
                            op0=ALU.add, op1=ALU.mult), reads=[cb_], writes=[cb_])
                k.barrier()

        evac_rr = [0]

        def evac_copy(out, in_, reads, writes):
            evac_rr[0] ^= 1
            if evac_rr[0]:
                k.op("act", lambda: nc.scalar.copy(out=out, in_=in_), reads=reads, writes=writes)
            else:
                k.op("dve", lambda: nc.vector.tensor_copy(out=out, in_=in_), reads=reads, writes=writes)

        def transpose_blocks(blocks, dst_fn, src_bufs, dst_buf):
            i = 0
            while i < len(blocks):
                n = min(8, len(blocks) - i)
                pt, pb = pT.next()
                w = blocks[i].shape[1]
                for t in range(n):
                    blk = blocks[i + t]
                    k.op("pe", lambda: nc.tensor.transpose(pt[0:w, t * 128:(t + 1) * 128], blk, ident[:]),
                         reads=list(src_bufs) + [cb_], writes=[pb], mark=(t == n - 1))
                evac_copy(dst_fn(i, n, w), pt[0:w, 0:n * 128].rearrange("p (n t) -> p n t", t=128), [pb], [dst_buf])
                i += n

        def rstd_of(ss, nh, hd, small):
            t1, b1 = small.next()
            k.op("dve", lambda: nc.vector.tensor_scalar(out=t1[:, :nh], in0=ss, scalar1=1.0 / hd, scalar2=EPS,
                                                        op0=ALU.mult, op1=ALU.add), reads=[ssb[0]], writes=[b1])
            k.op("act", lambda: nc.scalar.activation(out=t1[:, :nh], in_=t1[:, :nh], func=AF.Sqrt),
                 reads=[b1], writes=[b1])
            k.op("dve", lambda: nc.vector.reciprocal(out=t1[:, :nh], in_=t1[:, :nh]), reads=[b1], writes=[b1])
            return t1[:, :nh], b1

        ssb = [None]

        def load_hT(st, AT, ATb, src, row0, which, bufs):
            xt_ring, xn, xnb, small = bufs
            s = 0 if row0 < TS else 1
            for j in range(TG // 128):
                xt, xb = xt_ring.next()
                k.dma("sp", out=xt[:], in_=src[row0 + j * 128: row0 + (j + 1) * 128, :], writes=[xb])
                sst, sb_ = small.next()
                k.op("act", lambda: nc.scalar.activation(out=xn[:], in_=xt[:], func=AF.Square, accum_out=sst[:, 0:1]),
                     reads=[xb], writes=[xnb, sb_])
                ssb[0] = sb_
                rs, rb = rstd_of(sst[:, 0:1], 1, D, small)
                k.op("dve", lambda: nc.vector.tensor_scalar(out=xn[:], in0=xt[:], scalar1=rs[:, 0:1], scalar2=None,
                                                            op0=ALU.mult), reads=[xb, rb], writes=[xnb])
                for g in range(4):
                    pt, pb = pT.next()
                    for t in range(8):
                        kc = g * 8 + t
                        k.op("pe", lambda: nc.tensor.transpose(pt[:, t * 128:(t + 1) * 128],
                                                               xn[:, kc * 128:(kc + 1) * 128], ident[:]),
                             reads=[xnb, cb_], writes=[pb], mark=(t == 7))
                    for t in range(8):
                        kc = g * 8 + t
                        o = AT[:, kc, j * 128:(j + 1) * 128]
                        i_ = pt[:, t * 128:(t + 1) * 128]
                        sc_ = Gm[:, s, which, kc:kc + 1]
                        bi_ = fm[:, s, 2 * which + 1, kc:kc + 1]
                        if True:
                            k.op("act", lambda: nc.scalar.activation(out=o, in_=i_, func=AF.Identity, bias=bi_, scale=sc_),
                                 reads=[pb, cb_], writes=[ATb])
                        else:
                            k.op("dve", lambda: nc.vector.tensor_scalar(out=o, in0=i_, scalar1=sc_, scalar2=bi_,
                                                                        op0=ALU.mult, op1=ALU.add),
                                 reads=[pb, cb_], writes=[ATb])

        def load_AT(AT, ATb, src, row0, K, a_ring, col0=0):
            for j in range(TG // 128):
                at, ab = a_ring.next()
                k.dma("sp", out=at[:, :K], in_=src[row0 + j * 128: row0 + (j + 1) * 128, col0:col0 + K], writes=[ab])
                blocks = [at[:, kc * 128:(kc + 1) * 128] for kc in range(K // 128)]
                transpose_blocks(blocks, lambda i, n, w: AT[:, i:i + n, j * 128:(j + 1) * 128], [ab], ATb)

        def wload(wr, src, Kc, width):
            wt, wb = wr.next()
            wbuf = wb_of.get(src.name, wbuf0)
            if len(src.shape) == 2:
                src = src.rearrange("(kc p) n -> p kc n", p=128)
                k.dma("sp", out=wt[:, :Kc, :width], in_=src, reads=[wbuf], writes=[wb])
            else:
                a, b = src.shape[1], src.shape[2]
                for ai in range(a):
                    k.dma("sp", out=wt[:, :Kc, ai * b:(ai + 1) * b],
                          in_=src[:, ai, :].rearrange("(kc p) b -> p kc b", p=128), reads=[wbuf], writes=[wb])
            return wt, wb

        def gemm_run(AT, ATb, Kc, wt, wb, width, orient, pring, epi):
            for j in range(4):
                pt, pb = pring.next()
                for kc in range(Kc):
                    if orient == "A":
                        k.op("pe", lambda: nc.tensor.matmul(pt[:, :width], lhsT=AT[:, kc, j * 128:(j + 1) * 128],
                                                            rhs=wt[:, kc, :width], start=(kc == 0), stop=(kc == Kc - 1)),
                             reads=[ATb, wb], writes=[pb], mark=(kc == Kc - 1))
                    else:
                        k.op("pe", lambda: nc.tensor.matmul(pt[:, :TG], lhsT=wt[:, kc, j * 128:(j + 1) * 128],
                                                            rhs=AT[:, kc, :], start=(kc == 0), stop=(kc == Kc - 1)),
                             reads=[ATb, wb], writes=[pb], mark=(kc == Kc - 1))
                epi(j, pt, pb)

        def gemm_list(AT, ATb, Kc, jobs, wr, pring):
            def kc_of(job):
                return job[6] if len(job) > 4 else Kc
            nxt = wload(wr, jobs[0][0], kc_of(jobs[0]), jobs[0][1])
            for i, job in enumerate(jobs):
                src, width, orient, epi = job[:4]
                cur = nxt
                if i + 1 < len(jobs):
                    nxt = wload(wr, jobs[i + 1][0], kc_of(jobs[i + 1]), jobs[i + 1][1])
                if len(job) > 4:
                    gemm_run(job[4], job[5], job[6], cur[0], cur[1], width, orient, pring, epi)
                else:
                    gemm_run(AT, ATb, Kc, cur[0], cur[1], width, orient, pring, epi)

        def rmsnorm_heads(src3, srcbufs, nh, hd, g_bc, dst3, dstb, sq, sqb, small):
            k.op("act", lambda: nc.scalar.activation(out=sq[:, :nh * hd].rearrange("p (h d) -> p h d", d=hd), in_=src3,
                                                     func=AF.Square), reads=srcbufs, writes=[sqb])
            sst, sb_ = small.next()
            k.op("dve", lambda: nc.vector.tensor_reduce(out=sst[:, :nh], in_=sq[:, :nh * hd].rearrange("p (h d) -> p h d", d=hd),
                                                        axis=AX.X, op=ALU.add), reads=[sqb], writes=[sb_])
            ssb[0] = sb_
            rs, rb = rstd_of(sst[:, :nh], nh, hd, small)
            k.op("dve", lambda: nc.vector.tensor_tensor(out=dst3, in0=src3, in1=rs.unsqueeze(2).broadcast_to([128, nh, hd]),
                                                        op=ALU.mult), reads=list(srcbufs) + [rb], writes=[dstb])
            k.op("dve", lambda: nc.vector.tensor_tensor(out=dst3, in0=dst3, in1=g_bc.unsqueeze(1).broadcast_to([128, nh, hd]),
                                                        op=ALU.mult), reads=[dstb, cb_], writes=[dstb])

        def rope(t3, tb, nh, hd, cs, csb, tmp, tmpb):
            h2 = hd // 2
            v = t3.rearrange("p h (i two) -> p h i two", two=2)
            x1_, x2_ = v[:, :, :, 0], v[:, :, :, 1]
            cosb = cs[:, 0, :].unsqueeze(1).broadcast_to([128, nh, h2])
            sinb = cs[:, 1, :].unsqueeze(1).broadcast_to([128, nh, h2])
            ta = tmp[:, 0:nh * h2].rearrange("p (h i) -> p h i", i=h2)
            tb_ = tmp[:, nh * h2:2 * nh * h2].rearrange("p (h i) -> p h i", i=h2)
            tc_ = tmp[:, 2 * nh * h2:3 * nh * h2].rearrange("p (h i) -> p h i", i=h2)
            V = nc.vector
            k.op("dve", lambda: V.tensor_tensor(out=ta, in0=x1_, in1=sinb, op=ALU.mult), reads=[tb, csb], writes=[tmpb])
            k.op("dve", lambda: V.tensor_tensor(out=tb_, in0=x2_, in1=sinb, op=ALU.mult), reads=[tb, csb], writes=[tmpb])
            k.op("dve", lambda: V.tensor_tensor(out=x1_, in0=x1_, in1=cosb, op=ALU.mult), reads=[tb, csb], writes=[tb])
            k.op("dve", lambda: V.tensor_tensor(out=tc_, in0=x2_, in1=cosb, op=ALU.mult), reads=[tb, csb], writes=[tmpb])
            k.op("dve", lambda: V.tensor_tensor(out=x1_, in0=x1_, in1=tb_, op=ALU.subtract), reads=[tb, tmpb], writes=[tb])
            k.op("dve", lambda: V.tensor_tensor(out=x2_, in0=ta, in1=tc_, op=ALU.add), reads=[tmpb], writes=[tb])

        scrb = Buf(multi=True)
        if stop_after >= 2:
            with ExitStack() as ps:
                AT = sb(ps, "AT", [128, 32, TG], BF16)
                ATb = Buf(multi=True)
                xt_ring = Ring([sb(ps, "xt%d" % i, [128, D], F32) for i in range(2)])
                xn = sb(ps, "xn", [128, D], BF16)
                xnb = Buf()
                small = Ring([sb(ps, "sm%d" % i, [128, 8], F32) for i in range(8)])
                wr = Ring([sb(ps, "wr%d" % i, [128, 32, 512], BF16) for i in range(2)])
                qa_st = sb(ps, "qa_st", [128, 4, 1024], F32)
                qab = [Buf(multi=True) for _ in range(4)]
                st32 = Ring([sb(ps, "st32_%d" % i, [128, 1024], F32) for i in range(2)])
                sq = sb(ps, "sq", [128, 1024], F32)
                sqb = Buf()
                tmp = sb(ps, "tmp", [128, 768], F32)
                tmpb = Buf()
                st16 = Ring([sb(ps, "st16_%d" % i, [128, 1024], BF16) for i in range(3)])
                gT = Ring([sb(ps, "gT%d" % i, [128, 4, TG], BF16) for i in range(2)])
                gqa_bc = sb(ps, "gqa_bc", [128, 1024], F32)
                gkv_bc = sb(ps, "gkv_bc", [128, 512], F32)
                ggq_bc = sb(ps, "ggq_bc", [128, 128], F32)
                ggk_bc = sb(ps, "ggk_bc", [128, 128], F32)
                rm = sb(ps, "rm", [128, 4, 2, 32], F32)
                rg = sb(ps, "rg", [128, 4, 2, 64], F32)
                rb_ = Buf(multi=True)
                k.dma("sp", out=gqa_bc[:], in_=g_q_a.partition_broadcast(128), writes=[cb_])
                k.dma("sp", out=gkv_bc[:], in_=g_kv_a.partition_broadcast(128), writes=[cb_])
                k.dma("sp", out=ggq_bc[:], in_=g_gqa_q.partition_broadcast(128), writes=[cb_])
                k.dma("sp", out=ggk_bc[:], in_=g_gqa_k.partition_broadcast(128), writes=[cb_])
                pring = PRing([0, 1, 2, 3])
                col_blocks = [(0, 512), (512, 512), (1024, 512), (1536, 64), (1600, 512), (2112, 512), (2624, 512),
                              (3136, 512), (3648, 512), (4160, 512)]

                def cache_side():
                    for t in range(TC // 128):
                        kt = (TS // 128) + t
                        r0 = t * 128
                        s32, s32b = st32.next()
                        s16, s16b = st16.next()
                        k.dma("sp", out=s32[:, 0:512], in_=c_gk[r0:r0 + 128, :], writes=[s32b])
                        k.dma("sp", out=s32[:, 512:576], in_=c_kr[r0:r0 + 128, :], writes=[s32b])
                        evac_copy(s16[:, 0:576], s32[:, 0:576], [s32b], [s16b])
                        g_t, g_b = gT.next()
                        transpose_blocks([s16[:, h * 128:(h + 1) * 128] for h in range(4)],
                                         lambda i, n, w: g_t[:, i:i + n, 0:128], [s16b], g_b)
                        k.dma("pool", out=gkT[:, :, TS + r0:TS + r0 + 128].rearrange("h d t -> d h t"), in_=g_t[:, :, 0:128],
                              reads=[g_b], writes=[scrb])
                        g_t, g_b = gT.next()
                        transpose_blocks([s16[:, 512:576]], lambda i, n, w: g_t[0:64, 0:1, 0:128], [s16b], g_b)
                        k.dma("pool", out=kTr[:, TS + r0:TS + r0 + 128], in_=g_t[0:64, 0, 0:128], reads=[g_b], writes=[scrb])
                        s32, s32b = st32.next()
                        s16, s16b = st16.next()
                        k.dma("sp", out=s32[:, 0:512], in_=c_gv[r0:r0 + 128, :], writes=[s32b])
                        evac_copy(s16[:, 0:512], s32[:, 0:512], [s32b], [s16b])
                        k.dma("pool", out=gvm[:, :, kt, :].rearrange("h p d -> p h d"),
                              in_=s16[:, 0:512].rearrange("p (h d) -> p h d", d=128), reads=[s16b], writes=[scrb])

                if cfg.get('cache', True):
                    cache_side()

                for g in range(NTG):
                    row0 = g * TG
                    sample = row0 < TS
                    krow0 = row0 if sample else row0 + TC
                    flush_casts(6 if g == 0 else 3)
                    load_hT(ps, AT, ATb, xin, row0, 0, (xt_ring, xn, xnb, small))
                    if sample:
                        k.dma("sp", out=rm[:], in_=rope_m[row0:row0 + TG].rearrange("(j p) a i -> p j a i", p=128), writes=[rb_])
                        k.dma("sp", out=rg[:], in_=rope_g[row0:row0 + TG].rearrange("(j p) a i -> p j a i", p=128), writes=[rb_])
                    tr_state = {}

                    def make_epi(c):
                        c0, width = col_blocks[c]

                        def epi(j, pt, pb):
                            tok0 = row0 + j * 128
                            ktok0 = krow0 + j * 128
                            kt = ktok0 // 128
                            prow = tok0 - TS
                            if c in (0, 1):
                                evac_copy(qa_st[:, j, c * 512:(c + 1) * 512], pt[:, :512], [pb], [qab[j]])
                                if c == 1:
                                    s16, s16b = st16.next()
                                    s32, s32b = st32.next()
                                    rmsnorm_heads(qa_st[:, j:j + 1, :], [qab[j]], 1, 1024, gqa_bc[:],
                                                  s32[:, 0:1024].rearrange("p (h d) -> p h d", h=1), s32b, sq, sqb, small)
                                    evac_copy(s16[:, 0:1024], s32[:, 0:1024], [s32b], [s16b])
                                    k.dma("pool", out=qan[tok0:tok0 + 128, :], in_=s16[:, 0:1024], reads=[s16b], writes=[scrb])
                            elif c == 2:
                                s32, s32b = st32.next()
                                s16, s16b = st16.next()
                                rmsnorm_heads(pt[:, 0:512].rearrange("p (h d) -> p h d", h=1), [pb], 1, 512, gkv_bc[:],
                                              s32[:, 0:512].rearrange("p (h d) -> p h d", h=1), s32b, sq, sqb, small)
                                if not sample:
                                    k.dma("pool", out=n_ckv[prow:prow + 128, :], in_=s32[:, 0:512], reads=[s32b])
                                evac_copy(s16[:, 0:512], s32[:, 0:512], [s32b], [s16b])
                                k.dma("pool", out=ckv_s[ktok0:ktok0 + 128, :], in_=s16[:, 0:512], reads=[s16b], writes=[scrb])
                            elif c == 3:
                                s32, s32b = st32.next()
                                s16, s16b = st16.next()
                                evac_copy(s32[:, 0:64], pt[:, 0:64], [pb], [s32b])
                                if sample:
                                    rope(s32[:, 0:64].rearrange("p (h d) -> p h d", h=1), s32b, 1, 64, rm[:, j], rb_, tmp, tmpb)
                                else:
                                    k.dma("pool", out=n_kr[prow:prow + 128, :], in_=s32[:, 0:64], reads=[s32b])
                                evac_copy(s16[:, 0:64], s32[:, 0:64], [s32b], [s16b])
                                if j == 0:
                                    tr_state["kr"] = gT.next()
                                g_t, g_b = tr_state["kr"]
                                transpose_blocks([s16[:, 0:64]], lambda i, n, w: g_t[0:64, 0:1, j * 128:(j + 1) * 128], [s16b], g_b)
                                if j == 3:
                                    k.dma("pool", out=kTr[:, krow0:krow0 + TG], in_=g_t[0:64, 0, :], reads=[g_b], writes=[scrb])
                            elif c in (4, 5, 6, 7, 8):
                                isq = c < 8
                                s32, s32b = st32.next()
                                s16, s16b = st16.next()
                                d3 = s32[:, 0:512].rearrange("p (h d) -> p h d", d=128)
                                rmsnorm_heads(pt[:, 0:512].rearrange("p (h d) -> p h d", d=128), [pb], 4, 128,
                                              ggq_bc[:] if isq else ggk_bc[:], d3, s32b, sq, sqb, small)
                                if sample:
                                    rope(d3, s32b, 4, 128, rg[:, j], rb_, tmp, tmpb)
                                elif not isq:
                                    k.dma("pool", out=n_gk[prow:prow + 128, :], in_=s32[:, 0:512], reads=[s32b])
                                evac_copy(s16[:, 0:512], s32[:, 0:512], [s32b], [s16b])
                                if j == 0:
                                    tr_state[c] = gT.next()
                                g_t, g_b = tr_state[c]
                                transpose_blocks([s16[:, h * 128:(h + 1) * 128] for h in range(4)],
                                                 lambda i, n, w: g_t[:, i:i + n, j * 128:(j + 1) * 128], [s16b], g_b)
                                if j == 3:
                                    if isq:
                                        h0 = (c - 4) * 4
                                        k.dma("pool", out=gqT[h0:h0 + 4, :, row0:row0 + TG].rearrange("h d t -> d h t"), in_=g_t[:],
                                              reads=[g_b], writes=[scrb])
                                    else:
                                        k.dma("pool", out=gkT[:, :, krow0:krow0 + TG].rearrange("h d t -> d h t"), in_=g_t[:],
                                              reads=[g_b], writes=[scrb])
                            else:
                                s32, s32b = st32.next()
                                s16, s16b = st16.next()
                                evac_copy(s32[:, 0:512], pt[:, 0:512], [pb], [s32b])
                                if not sample:
                                    k.dma("pool", out=n_gv[prow:prow + 128, :], in_=s32[:, 0:512], reads=[s32b])
                                evac_copy(s16[:, 0:512], s32[:, 0:512], [s32b], [s16b])
                                k.dma("pool", out=gvm[:, :, kt, :].rearrange("h p d -> p h d"),
                                      in_=s16[:, 0:512].rearrange("p (h d) -> p h d", d=128), reads=[s16b], writes=[scrb])
                        return epi

                    jobs = [(w_in_b[:, c0:c0 + wd], wd, "A", make_epi(c)) for c, (c0, wd) in enumerate(col_blocks) if c in cfg.get('ablk', range(10))]
                    if jobs:
                        gemm_list(AT, ATb, 32, jobs, wr, pring)
                k.barrier()

        pass

        if stop_after >= 3:
            with ExitStack() as ps:
                AT = sb(ps, "AT2", [128, 8, TG], BF16)
                ATb = Buf(multi=True)
                a_ring = Ring([sb(ps, "a2_%d" % i, [128, 1024], BF16) for i in range(2)])
                wr = Ring([sb(ps, "wr2_%d" % i, [128, 8, 512], BF16) for i in range(2)], multi=True)
                st16 = Ring([sb(ps, "s16_%d" % i, [128, 512], BF16) for i in range(3)])
                st32 = Ring([sb(ps, "s32_%d" % i, [128, 512], F32) for i in range(2)])
                tmp = sb(ps, "tmp2", [128, 768], F32)
                tmpb = Buf()
                qr_st = Ring([sb(ps, "qr_st%d" % i, [64, 8, TG], BF16) for i in range(2)])
                rm = sb(ps, "rm2", [128, 4, 2, 32], F32)
                rb_ = Buf(multi=True)
                pring = PRing([0, 1, 2, 3])
                wq3 = w_q_bb.rearrange("k (h c) -> k h c", c=192)
                wkv3 = w_kv_bb.rearrange("k (h c) -> k h c", c=256)
                for g in range(NTG):
                    row0 = g * TG
                    sample = row0 < TS
                    load_AT(AT, ATb, qan, row0, 1024, a_ring)
                    if sample:
                        k.dma("sp", out=rm[:], in_=rope_m[row0:row0 + TG].rearrange("(j p) a i -> p j a i", p=128), writes=[rb_])
                    jobs = []
                    for h0 in range(0, 16, 4):
                        def epi_n(j, pt, pb, h0=h0):
                            s16, s16b = st16.next()
                            evac_copy(s16[:, 0:TG], pt[:, 0:TG], [pb], [s16b])
                            k.dma("pool", out=qTn[h0 + j, :, row0:row0 + TG], in_=s16[:, 0:TG], reads=[s16b], writes=[scrb])
                        jobs.append((wq3[:, h0:h0 + 4, 0:128], 512, "B", epi_n))
                    for h0 in range(0, 16, 8):
                        st = {}
                        def epi_r(j, pt, pb, h0=h0, st=st):
                            s32, s32b = st32.next()
                            s16, s16b = st16.next()
                            evac_copy(s32[:, 0:512], pt[:, 0:512], [pb], [s32b])
                            if sample:
                                rope(s32[:, 0:512].rearrange("p (h d) -> p h d", d=64), s32b, 8, 64, rm[:, j], rb_, tmp, tmpb)
                            evac_copy(s16[:, 0:512], s32[:, 0:512], [s32b], [s16b])
                            if j == 0:
                                st["t"] = qr_st.next()
                            q_t, q_b = st["t"]
                            transpose_blocks([s16[:, hh * 64:(hh + 1) * 64] for hh in range(8)],
                                             lambda i, n, w: q_t[0:64, i:i + n, j * 128:(j + 1) * 128], [s16b], q_b)
                            if j == 3:
                                k.dma("pool", out=qTr[h0:h0 + 8, :, row0:row0 + TG].rearrange("h d t -> d h t"), in_=q_t[:],
                                      reads=[q_b], writes=[scrb])
                        jobs.append((wq3[:, h0:h0 + 8, 128:192], 512, "A", epi_r))
                    gemm_list(AT, ATb, 8, jobs, wr, pring)
                for g in range(NKG):
                    krow0 = g * TG
                    load_AT(AT, ATb, ckv_s, krow0, 512, a_ring)
                    jobs = []
                    for h0 in range(0, 16, 4):
                        def epi_kn(j, pt, pb, h0=h0):
                            s16, s16b = st16.next()
                            evac_copy(s16[:, 0:TG], pt[:, 0:TG], [pb], [s16b])
                            k.dma("pool", out=kTn[h0 + j, :, krow0:krow0 + TG], in_=s16[:, 0:TG], reads=[s16b], writes=[scrb])
                        jobs.append((wkv3[:, h0:h0 + 4, 0:128], 512, "B", epi_kn))
                    for h0 in range(0, 16, 4):
                        def epi_v(j, pt, pb, h0=h0):
                            s16, s16b = st16.next()
                            evac_copy(s16[:, 0:512], pt[:, 0:512], [pb], [s16b])
                            kt = krow0 // 128 + j
                            k.dma("pool", out=vm[h0:h0 + 4, :, kt, :].rearrange("h p d -> p h d"),
                                  in_=s16[:, 0:512].rearrange("p (h d) -> p h d", d=128), reads=[s16b], writes=[scrb])
                        jobs.append((wkv3[:, h0:h0 + 4, 128:256], 512, "A", epi_v))
                    gemm_list(AT, ATb, 4, jobs, wr, pring)
                k.barrier()

        if stop_after >= 4:
            with ExitStack() as ps:
                MAXK = (TS + TC)
                qn = Ring([sb(ps, "qn%d" % i, [128, TS], BF16) for i in range(2)])
                qr = Ring([sb(ps, "qr%d" % i, [64, TS], BF16) for i in range(2)])
                kn = Ring([sb(ps, "kn%d" % i, [128, MAXK], BF16) for i in range(2)])
                kr_ = Ring([sb(ps, "kr%d" % i, [64, MAXK], BF16) for i in range(2)])
                vv = Ring([sb(ps, "vv%d" % i, [128, MAXK // 128, 130], BF16) for i in range(2)])
                pp = Ring([sb(ps, "pp%d" % i, [128, 512], BF16) for i in range(4)])
                ost = Ring([sb(ps, "ost%d" % i, [128, 128], BF16) for i in range(4)])
                rcp = Ring([sb(ps, "rcp%d" % i, [128, 1], F32) for i in range(4)])
                class SRing:
                    def __init__(self):
                        self.t = [pf[0], pf[1], ptb[0][:].bitcast(F32), ptb[1][:].bitcast(F32)]
                        self.b = [pfb[0], pfb[1], pT.bufs[0], pT.bufs[1]]
                        self.i = 0

                    def next(self):
                        j = self.i
                        self.i = (self.i + 1) % 4
                        return self.t[j], self.b[j]
                sring = SRing()
                for (q0, nq, k0, nk) in seqs:
                    nkt = nk // 128
                    for typ in ("mla", "gqa"):
                        for h in range(16):
                            flush_casts(1)
                            qn_t, qn_b = qn.next()
                            kn_t, kn_b = kn.next()
                            v_t, v_b = vv.next()
                            if typ == "mla":
                                qr_t, qr_b = qr.next()
                                kr_t, kr_b = kr_.next()
                                k.dma("sp", out=qn_t[:, :nq], in_=qTn[h, :, q0:q0 + nq], reads=[scrb], writes=[qn_b])
                                k.dma("sp", out=qr_t[:, :nq], in_=qTr[h, :, q0:q0 + nq], reads=[scrb], writes=[qr_b])
                                k.dma("sp", out=kn_t[:, :nk], in_=kTn[h, :, k0:k0 + nk], reads=[scrb], writes=[kn_b])
                                k.dma("sp", out=kr_t[:, :nk], in_=kTr[:, k0:k0 + nk], reads=[scrb], writes=[kr_b])
                                k.dma("sp", out=v_t[:, :nkt, 0:128], in_=vm[h, :, k0 // 128:k0 // 128 + nkt, :], reads=[scrb], writes=[v_b])
                                scale = 192.0 ** -0.5
                                odst = o_mla
                            else:
                                k.dma("sp", out=qn_t[:, :nq], in_=gqT[h, :, q0:q0 + nq], reads=[scrb], writes=[qn_b])
                                k.dma("sp", out=kn_t[:, :nk], in_=gkT[h // 4, :, k0:k0 + nk], reads=[scrb], writes=[kn_b])
                                k.dma("sp", out=v_t[:, :nkt, 0:128], in_=gvm[h // 4, :, k0 // 128:k0 // 128 + nkt, :], reads=[scrb], writes=[v_b])
                                scale = 128.0 ** -0.5
                                odst = o_gqa
                            k.op("dve", lambda: nc.vector.memset(v_t[:, :nkt, 128:129], 1.0), writes=[v_b])
                            for qg0 in range(0, nq, 512):
                                qw = min(512, nq - qg0)
                                nqs = qw // 128
                                def emit_S(kt):
                                    st_, sb__ = sring.next()
                                    if typ == "mla":
                                        k.op("pe", lambda: nc.tensor.matmul(st_[:, :qw], lhsT=kn_t[:, kt * 128:(kt + 1) * 128],
                                                                            rhs=qn_t[:, qg0:qg0 + qw], start=True, stop=False),
                                             reads=[kn_b, qn_b], writes=[sb__], mark=False)
                                        k.op("pe", lambda: nc.tensor.matmul(st_[:, :qw], lhsT=kr_t[:, kt * 128:(kt + 1) * 128],
                                                                            rhs=qr_t[:, qg0:qg0 + qw], start=False, stop=True),
                                             reads=[kr_b, qr_b], writes=[sb__])
                                    else:
                                        k.op("pe", lambda: nc.tensor.matmul(st_[:, :qw], lhsT=kn_t[:, kt * 128:(kt + 1) * 128],
                                                                            rhs=qn_t[:, qg0:qg0 + qw], start=True, stop=True),
                                             reads=[kn_b, qn_b], writes=[sb__])
                                    return st_, sb__

                                LA = 2
                                pend = [emit_S(kt0) for kt0 in range(min(LA, nkt))]
                                for kt in range(nkt):
                                    st_, sb__ = pend.pop(0)
                                    p_t, p_b = pp.next()
                                    k.op("act", lambda: nc.scalar.activation(out=p_t[:, :qw], in_=st_[:, :qw], func=AF.Exp, scale=scale),
                                         reads=[sb__], writes=[p_b])
                                    if kt + LA < nkt:
                                        pend.append(emit_S(kt + LA))
                                    for qs in range(nqs):
                                        k.op("pe", lambda: nc.tensor.matmul(pf[2 + qs][:, 0:129], lhsT=p_t[:, qs * 128:(qs + 1) * 128],
                                                                            rhs=v_t[:, kt, 0:129], start=(kt == 0), stop=(kt == nkt - 1)),
                                             reads=[p_b, v_b], writes=[pfb[2 + qs]], mark=(kt == nkt - 1 or qs == nqs - 1))
                                for qs in range(nqs):
                                    r_t, r_b = rcp.next()
                                    o_t, o_b = ost.next()
                                    k.op("dve", lambda: nc.vector.reciprocal(out=r_t[:], in_=pf[2 + qs][:, 128:129]),
                                         reads=[pfb[2 + qs]], writes=[r_b])
                                    k.op("dve", lambda: nc.vector.tensor_scalar(out=o_t[:], in0=pf[2 + qs][:, 0:128], scalar1=r_t[:, 0:1],
                                                                                scalar2=None, op0=ALU.mult),
                                         reads=[pfb[2 + qs], r_b], writes=[o_b])
                                    r0 = q0 + qg0 + qs * 128
                                    k.dma("pool", out=odst[r0:r0 + 128, h * 128:(h + 1) * 128], in_=o_t[:], reads=[o_b], writes=[scrb])
                k.barrier()

        flush_casts(1000)
        utb = Buf(multi=True)
        if stop_after >= 5:
            with ExitStack() as ps:
                ATh = sb(ps, "ATh", [128, 32, TG], BF16)
                AThb = Buf(multi=True)
                ATm = sb(ps, "ATm", [128, 16, TG], BF16)
                ATmb = Buf(multi=True)
                ATg = sb(ps, "ATg", [128, 16, TG], BF16)
                ATgb = Buf(multi=True)
                xt_ring = Ring([sb(ps, "xtc", [128, D], F32)])
                xn = sb(ps, "xnc", [128, D], BF16)
                xnb = Buf()
                small = Ring([sb(ps, "smc%d" % i, [128, 8], F32) for i in range(6)])
                a_ring = Ring([sb(ps, "ac_%d" % i, [128, 2048], BF16) for i in range(2)])
                wr = Ring([sb(ps, "wrc%d" % i, [128, 32, 512], BF16) for i in range(2)])
                sg = sb(ps, "sg", [128, 4, 512], F32)
                sgb = [Buf() for _ in range(4)]
                acc = sb(ps, "accc", [128, 4, 512], F32)
                accb = [Buf() for _ in range(4)]
                tt = Ring([sb(ps, "ttc%d" % i, [128, 512], F32) for i in range(2)])
                o16 = Ring([sb(ps, "o16c%d" % i, [128, 512], BF16) for i in range(3)])
                pring = PRing([0, 1, 2, 3])
                mergedb = Buf(multi=True)
                for g in range(NTG):
                    row0 = g * TG
                    load_hT(ps, ATh, AThb, xin, row0, 0, (xt_ring, xn, xnb, small))
                    load_AT(ATm, ATmb, o_mla, row0, 2048, a_ring)
                    load_AT(ATg, ATgb, o_gqa, row0, 2048, a_ring)
                    jobs = []
                    for cb in range(8):
                        c0 = cb * 512

                        def epi_g(j, pt, pb):
                            k.op("act", lambda: nc.scalar.activation(out=sg[:, j, :], in_=pt[:, 0:512], func=AF.Sigmoid),
                                 reads=[pb], writes=[sgb[j]])

                        def epi_m1(j, pt, pb):
                            k.op("dve", lambda: nc.vector.tensor_tensor(out=acc[:, j, :], in0=pt[:, 0:512], in1=sg[:, j, :], op=ALU.mult),
                                 reads=[pb, sgb[j]], writes=[accb[j]])

                        def epi_m2(j, pt, pb, c0=c0):
                            t_t, t_b = tt.next()
                            o_t, o_b = o16.next()
                            k.op("dve", lambda: nc.vector.tensor_tensor(out=t_t[:], in0=pt[:, 0:512], in1=sg[:, j, :], op=ALU.mult),
                                 reads=[pb, sgb[j]], writes=[t_b])
                            k.op("dve", lambda: nc.vector.tensor_tensor(out=o_t[:], in0=t_t[:], in1=acc[:, j, :], op=ALU.add),
                                 reads=[t_b, accb[j]], writes=[o_b])
                            tok0 = row0 + j * 128
                            k.dma("pool", out=merged[tok0:tok0 + 128, c0:c0 + 512], in_=o_t[:], reads=[o_b], writes=[mergedb])

                        jobs.append((w_in_b[:, 4672 + c0:4672 + c0 + 512], 512, "A", epi_g, ATh, AThb, 32))
                        jobs.append((w_mla_ob[:, c0:c0 + 512], 512, "A", epi_m1, ATm, ATmb, 16))
                        jobs.append((w_in_b[:, 8768 + c0:8768 + c0 + 512], 512, "A", epi_g, ATh, AThb, 32))
                        jobs.append((w_gqa_ob[:, c0:c0 + 512], 512, "A", epi_m2, ATg, ATgb, 16))
                    gemm_list(None, None, None, jobs, wr, pring)
                k.barrier()
            with ExitStack() as ps:
                AT = sb(ps, "ATc2", [128, 32, TG], BF16)
                ATb = Buf(multi=True)
                a_ring = Ring([sb(ps, "ac2_%d" % i, [128, D], BF16) for i in range(2)])
                wr = Ring([sb(ps, "wrc2%d" % i, [128, 32, 512], BF16) for i in range(2)])
                g1bc = sb(ps, "g1bc", [128, D], F32)
                g1b = Buf()
                xs = Ring([sb(ps, "xs%d" % i, [128, 512], F32) for i in range(3)])
                tt = Ring([sb(ps, "ttd%d" % i, [128, 512], F32) for i in range(3)])
                pring = PRing([0, 1, 2, 3])
                x1b = Buf(multi=True)
                cur_set = None
                do_u = stop_after >= 7
                if do_u:
                    uring = Ring([sb(ps, "ur%d" % i, [128, D], F32) for i in range(2)])
                    ustr = Ring([sb(ps, "us%d" % i, [128, 32, 128], BF16) for i in range(2)], multi=True)
                    upr = PRing([4, 5])
                ust = {"done": 0, "calls": 0}
                total_calls = NTG * 8 * 4

                def uprep(eb):
                    ut, ub = uring.next()
                    k.dma("sp", out=ut[:], in_=peer_u[eb * 128:(eb + 1) * 128, :], writes=[ub])
                    us, usb = ustr.next()
                    for g4 in range(8):
                        pt, pb = upr.next()
                        for t in range(4):
                            dc = g4 * 4 + t
                            k.op("pe", lambda: nc.tensor.transpose(pt[:, t * 128:(t + 1) * 128], ut[:, dc * 128:(dc + 1) * 128], ident_f[:]),
                                 reads=[ub, cb_], writes=[pb], mark=(t == 3))
                        evac_copy(us[:, g4 * 4:(g4 + 1) * 4, :], pt[:, 0:512].rearrange("p (t e) -> p t e", e=128), [pb], [usb])
                    k.dma("pool", out=ut_b[eb], in_=us[:], reads=[usb], writes=[utb])

                def uprep_tick():
                    if not do_u:
                        return
                    ust["calls"] += 1
                    while ust["done"] < 128 and ust["done"] * total_calls < ust["calls"] * 128:
                        uprep(ust["done"])
                        ust["done"] += 1
                for g in range(NTG):
                    row0 = g * TG
                    sset = 0 if row0 < TS else 1
                    if sset != cur_set:
                        k.dma("sp", out=g1bc[:], in_=mods[sset, 2 * D:3 * D].partition_broadcast(128), reads=[modb], writes=[g1b])
                        cur_set = sset
                    load_AT(AT, ATb, merged, row0, D, a_ring)
                    jobs = []
                    for cb in range(8):
                        c0 = cb * 512

                        def epi_o(j, pt, pb, c0=c0):
                            tok0 = row0 + j * 128
                            x_t, x_b = xs.next()
                            t_t, t_b = tt.next()
                            k.dma("sp", out=x_t[:], in_=xin[tok0:tok0 + 128, c0:c0 + 512], writes=[x_b])
                            k.op("dve", lambda: nc.vector.tensor_tensor(out=t_t[:], in0=pt[:, 0:512], in1=g1bc[:, c0:c0 + 512], op=ALU.mult),
                                 reads=[pb, g1b], writes=[t_b])
                            k.op("dve", lambda: nc.vector.tensor_tensor(out=t_t[:], in0=t_t[:], in1=x_t[:], op=ALU.add),
                                 reads=[t_b, x_b], writes=[t_b])
                            k.dma("pool", out=x1[tok0:tok0 + 128, c0:c0 + 512], in_=t_t[:], reads=[t_b], writes=[x1b])
                            uprep_tick()

                        jobs.append((w_out_b[:, c0:c0 + 512], 512, "A", epi_o))
                    gemm_list(AT, ATb, 32, jobs, wr, pring)
                k.barrier()

        if stop_after >= 6 or only == "d1b":
            with ExitStack() as ps:
                AT = sb(ps, "ATd", [128, 32, TG], BF16)
                ATb = Buf(multi=True)
                xt_ring = Ring([sb(ps, "xtd", [128, D], F32)])
                xn = sb(ps, "xnd", [128, D], BF16)
                xnb = Buf()
                small = Ring([sb(ps, "smd%d" % i, [128, 8], F32) for i in range(6)])
                wr = Ring([sb(ps, "wrd%d" % i, [128, 32, 512], BF16) for i in range(2)])
                st16 = Ring([sb(ps, "s16d_%d" % i, [128, 512], BF16) for i in range(3)])
                pring = PRing([0, 1, 2, 3])
                qpb = Buf(multi=True)
                for g in range(0 if only else NTG):
                    row0 = g * TG
                    load_hT(ps, AT, ATb, x1, row0, 1, (xt_ring, xn, xnb, small))
                    jobs = []
                    for c in range(4):
                        def epi_q(j, pt, pb, c=c):
                            s16, s16b = st16.next()
                            evac_copy(s16[:, 0:TG], pt[:, 0:TG], [pb], [s16b])
                            k.dma("pool", out=qpT[c * 4 + j, :, row0:row0 + TG], in_=s16[:, 0:TG], reads=[s16b], writes=[qpb])
                        jobs.append((w_pq_b[:, c * 512:(c + 1) * 512], 512, "B", epi_q))
                    gemm_list(AT, ATb, 32, jobs, wr, pring)
                k.barrier()

            with ExitStack() as ps:
                V = nc.vector
                skf = sb(ps, "skf", [128, 2, 128], F32)
                skT = sb(ps, "skT", [128, 2, 128], BF16)
                skb = Buf()
                qt_ring = Ring([sb(ps, "qt%d" % i, [128, 16, 128], BF16) for i in range(2)])
                S = sb(ps, "S", [128, 16, 128], F32)
                Sb = Buf()
                S2 = sb(ps, "S2", [128, 16, 128], F32)
                S2b = Buf()
                sv = sb(ps, "sv", [128, 16, 16], F32)
                si_u = sb(ps, "si_u", [128, 16, 16], U32)
                si_f = sb(ps, "si_f", [128, 16, 16], F32)
                svb = Buf()
                cand = sb(ps, "cand", [128, 8, 256], F32)
                cand2 = S2[:].rearrange("p a m -> p (a m)").rearrange("p (h c) -> p h c", c=256)
                candb = Buf()
                bv = sb(ps, "bv", [128, 8, 16], F32)
                bp_u = sb(ps, "bp_u", [128, 8, 16], U32)
                il_u = sb(ps, "il_u", [128, 8, 16], U32)
                jl_u = sb(ps, "jl_u", [128, 8, 16], U32)
                il_f = sb(ps, "il_f", [128, 8, 16], F32)
                jl_f = sb(ps, "jl_f", [128, 8, 16], F32)
                bvb = Buf()
                oh = S[:].rearrange("p a m -> p (a m)").rearrange("p (h a l) -> p h a l", h=8, a=16)
                ohb = Sb
                sel = sb(ps, "sel", [128, 3, 128], F32)
                selb = Buf()
                zz = sb(ps, "zz", [128, 8], F32)
                T3 = sb(ps, "T3", [128, 3, 128], F32)
                T3b = Buf()
                Cring = Ring([sb(ps, "Cm%d" % i, [128, 128, 128], BF16) for i in range(2)])
                Rring = Ring([sb(ps, "Rm%d" % i, [128, 128, 128], BF16) for i in range(2)])
                Gst = Ring([sb(ps, "Gst%d" % i, [128, 128, 128], BF16) for i in range(1)], multi=True)
                Gsb = Buf(multi=True)
                gring = PRing([4, 5])
                k.dma("sp", out=skf[:], in_=sub_keys.rearrange("p m k -> m p k"), writes=[skb])
                ptk, pbk = pf[0], pfb[0]
                for p_ in range(2):
                    k.op("pe", lambda: nc.tensor.transpose(ptk[:, p_ * 128:(p_ + 1) * 128], skf[:, p_, :], ident_f[:]),
                         reads=[skb, cb_], writes=[pbk], mark=(p_ == 1))
                k.op("dve", lambda: V.tensor_copy(out=skT[:].rearrange("p a m -> p (a m)"), in_=ptk[:, 0:256]), reads=[pbk], writes=[skb])
                iota16 = iota_f[:, 0:16].unsqueeze(1).unsqueeze(1).broadcast_to([128, 8, 16, 16])
                def emit_scores(nt):
                    qt, qb = qt_ring.next()
                    k.dma("sp", out=qt[:], in_=qpT[:, :, nt * 128:(nt + 1) * 128].rearrange("a k t -> k a t"), reads=[qpb], writes=[qb])
                    for pair in range(16):
                        bank = pair // 4
                        k.op("pe", lambda: nc.tensor.matmul(pf[bank][:, (pair % 4) * 128:(pair % 4 + 1) * 128], lhsT=qt[:, pair, :],
                                                            rhs=skT[:, pair % 2, :], start=True, stop=True),
                             reads=[qb, skb], writes=[pfb[bank]], mark=(pair % 4 == 3))
                    for bank in range(4):
                        k.op("act", lambda: nc.scalar.copy(out=S[:, bank * 4:(bank + 1) * 4, :],
                                                           in_=pf[bank][:, 0:512].rearrange("p (a m) -> p a m", m=128)),
                             reads=[pfb[bank]], writes=[Sb])

                n_tiles = cfg.get('d1b_nt', NQT)
                if n_tiles > 0:
                    emit_scores(0)
                for nt in range(n_tiles):
                    for pair in range(16):
                        k.op("dve", lambda: V.max(out=sv[:, pair, 0:8], in_=S[:, pair, :]), reads=[Sb], writes=[svb])
                        k.op("dve", lambda: V.max_index(out=si_u[:, pair, 0:8], in_max=sv[:, pair, 0:8], in_values=S[:, pair, :]),
                             reads=[Sb, svb], writes=[svb])
                        k.op("dve", lambda: V.match_replace(out=S2[:, pair, :], in_to_replace=sv[:, pair, 0:8], in_values=S[:, pair, :],
                                                            imm_value=NEG), reads=[Sb, svb], writes=[S2b])
                        k.op("dve", lambda: V.max(out=sv[:, pair, 8:16], in_=S2[:, pair, :]), reads=[S2b], writes=[svb])
                        k.op("dve", lambda: V.max_index(out=si_u[:, pair, 8:16], in_max=sv[:, pair, 8:16], in_values=S2[:, pair, :]),
                             reads=[S2b, svb], writes=[svb])
                    k.op("dve", lambda: V.tensor_copy(out=si_f[:], in_=si_u[:]), reads=[svb], writes=[svb])
                    sv4 = sv[:].rearrange("p (h a) t -> p h a t", a=2)
                    si4 = si_f[:].rearrange("p (h a) t -> p h a t", a=2)
                    c4 = cand[:].rearrange("p h (i j) -> p h i j", j=16)
                    k.op("dve", lambda: V.tensor_tensor(out=c4, in0=sv4[:, :, 0, :].unsqueeze(3).broadcast_to([128, 8, 16, 16]),
                                                        in1=sv4[:, :, 1, :].unsqueeze(2).broadcast_to([128, 8, 16, 16]), op=ALU.add),
                         reads=[svb], writes=[candb])
                    for h in range(8):
                        k.op("dve", lambda: V.max(out=bv[:, h, 0:8], in_=cand[:, h, :]), reads=[candb], writes=[bvb])
                        k.op("dve", lambda: V.max_index(out=bp_u[:, h, 0:8], in_max=bv[:, h, 0:8], in_values=cand[:, h, :]),
                             reads=[candb, bvb], writes=[bvb])
                        k.op("dve", lambda: V.match_replace(out=cand2[:, h, :], in_to_replace=bv[:, h, 0:8], in_values=cand[:, h, :],
                                                            imm_value=NEG), reads=[candb, bvb], writes=[S2b])
                        k.op("dve", lambda: V.max(out=bv[:, h, 8:16], in_=cand2[:, h, :]), reads=[S2b], writes=[bvb])
                        k.op("dve", lambda: V.max_index(out=bp_u[:, h, 8:16], in_max=bv[:, h, 8:16], in_values=cand2[:, h, :]),
                             reads=[S2b, bvb], writes=[bvb])
                    k.op("dve", lambda: V.tensor_single_scalar(out=il_u[:], in_=bp_u[:], scalar=4, op=ALU.logical_shift_right),
                         reads=[bvb], writes=[bvb])
                    k.op("dve", lambda: V.tensor_single_scalar(out=jl_u[:], in_=bp_u[:], scalar=15, op=ALU.bitwise_and),
                         reads=[bvb], writes=[bvb])
                    k.op("dve", lambda: V.tensor_copy(out=il_f[:], in_=il_u[:]), reads=[bvb], writes=[bvb])
                    k.op("dve", lambda: V.tensor_copy(out=jl_f[:], in_=jl_u[:]), reads=[bvb], writes=[bvb])
                    for a_, (lf, dsti) in enumerate(((il_f, 0), (jl_f, 1))):
                        k.op("dve", lambda: V.tensor_tensor(out=oh[:], in0=lf[:].unsqueeze(3).broadcast_to([128, 8, 16, 16]), in1=iota16,
                                                            op=ALU.is_equal), reads=[bvb, cb_], writes=[ohb])
                        k.op("dve", lambda: V.tensor_tensor(out=oh[:], in0=oh[:], in1=si4[:, :, a_, :].unsqueeze(2).broadcast_to([128, 8, 16, 16]),
                                                            op=ALU.mult), reads=[ohb, svb], writes=[ohb])
                        k.op("dve", lambda: V.tensor_reduce(out=sel[:, dsti, :].rearrange("p (h a) -> p h a", a=16), in_=oh[:], axis=AX.X, op=ALU.add),
                             reads=[ohb], writes=[selb])
                    g3 = sel[:, 2, :].rearrange("p (h a) -> p h a", a=16)
                    k.op("dve", lambda: V.tensor_tensor(out=g3, in0=bv[:], in1=bv[:, :, 0:1].broadcast_to([128, 8, 16]), op=ALU.subtract),
                         reads=[bvb], writes=[selb])
                    k.op("act", lambda: nc.scalar.activation(out=g3, in_=g3, func=AF.Exp), reads=[selb], writes=[selb])
                    k.op("dve", lambda: V.tensor_reduce(out=zz[:], in_=g3, axis=AX.X, op=ALU.add), reads=[selb], writes=[ohb])
                    k.op("dve", lambda: V.reciprocal(out=zz[:], in_=zz[:]), reads=[ohb], writes=[ohb])
                    k.op("dve", lambda: V.tensor_tensor(out=g3, in0=g3, in1=zz[:].unsqueeze(2).broadcast_to([128, 8, 16]), op=ALU.mult),
                         reads=[selb, ohb], writes=[selb])
                    ptt, pbt = gring.next()
                    for a_ in range(3):
                        k.op("pe", lambda: nc.tensor.transpose(ptt[:, a_ * 128:(a_ + 1) * 128], sel[:, a_, :], ident_f[:]),
                             reads=[selb, cb_], writes=[pbt], mark=(a_ == 2))
                    k.op("dve", lambda: V.tensor_copy(out=T3[:].rearrange("p a t -> p (a t)"), in_=ptt[:, 0:384]), reads=[pbt], writes=[T3b])
                    iob = iota_f[:].unsqueeze(1).broadcast_to([128, 128, 128])
                    Cm, Cb = Cring.next()
                    Rm, Rb = Rring.next()
                    k.op("dve", lambda: V.tensor_tensor(out=Cm[:], in0=iob, in1=T3[:, 1, :].unsqueeze(2).broadcast_to([128, 128, 128]),
                                                        op=ALU.is_equal), reads=[T3b, cb_], writes=[Cb])
                    k.op("dve", lambda: V.tensor_tensor(out=Rm[:], in0=iob, in1=T3[:, 0, :].unsqueeze(2).broadcast_to([128, 128, 128]),
                                                        op=ALU.is_equal), reads=[T3b, cb_], writes=[Rb])
                    k.op("pool", lambda: nc.gpsimd.tensor_tensor(out=Rm[:], in0=Rm[:], in1=T3[:, 2, :].unsqueeze(2).broadcast_to([128, 128, 128]),
                                                                 op=ALU.mult), reads=[T3b, Rb], writes=[Rb])
                    if nt + 1 < n_tiles:
                        emit_scores(nt + 1)
                    gst, gstb = Gst.next()
                    for n0 in range(0, 128, 4):
                        ptg, pbg = gring.next()
                        for t in range(4):
                            n = n0 + t
                            k.op("pe", lambda: nc.tensor.matmul(ptg[:, t * 128:(t + 1) * 128], lhsT=Cm[:, n, :], rhs=Rm[:, n, :],
                                                                start=True, stop=True), reads=[Cb, Rb], writes=[pbg], mark=(t == 3))
                        k.op("act", lambda: nc.scalar.copy(out=gst[:, :, n0:n0 + 4], in_=ptg[:, 0:512].rearrange("p (n i) -> p i n", i=128)),
                             reads=[pbg], writes=[gstb])
                    k.dma("pool", out=Gs[nt], in_=gst[:], reads=[gstb], writes=[Gsb])
                k.barrier()

        if stop_after >= 7:
            with ExitStack() as ps:
                V = nc.vector
                AT = sb(ps, "ATe", [128, 32, TG], BF16)
                ATb = Buf(multi=True)
                xt_ring = Ring([sb(ps, "xte", [128, D], F32)])
                xn = sb(ps, "xne", [128, D], BF16)
                xnb = Buf()
                small = Ring([sb(ps, "sme%d" % i, [128, 8], F32) for i in range(6)])
                acc = sb(ps, "acce", [128, 4, D], F32)
                accb = [Buf() for _ in range(4)]
                utr = Ring([sb(ps, "utr%d" % i, [128, 32, 128], BF16) for i in range(2)])
                vr = Ring([sb(ps, "vr%d" % i, [128, D], BF16) for i in range(5)])
                wT = Ring([sb(ps, "wT%d" % i, [128, 4, TG], BF16) for i in range(2)], multi=True)
                ge = Ring([sb(ps, "ge%d" % i, [128, TG], F32) for i in range(2)])
                gt = Ring([sb(ps, "gt%d" % i, [128, 4, 128], BF16) for i in range(3)])
                gch = Ring([sb(ps, "gch%d" % i, [128, 1024], F32) for i in range(2)])
                aring = PRing([0, 1])
                oring = PRing([2, 3, 4, 5])
                EG = 4
                for g in range(NTG):
                    row0 = g * TG
                    sset = 0 if row0 < TS else 1
                    load_hT(ps, AT, ATb, x1, row0, 1, (xt_ring, xn, xnb, small))
                    for eg in range(128 // EG):
                        w_t, w_b = wT.next()
                        vts = []
                        for e_ in range(EG):
                            i = eg * EG + e_
                            u_t, u_b = utr.next()
                            k.dma("sp", out=u_t[:], in_=ut_b[i], reads=[utb], writes=[u_b])
                            g_t, g_b = gt.next()
                            k.dma("sp", out=g_t[:], in_=Gs[g * 4:(g + 1) * 4, :, i, :].rearrange("t j n -> j t n"), reads=[Gsb], writes=[g_b])
                            v_t, v_b = vr.next()
                            k.dma("sp", out=v_t[:], in_=pv_b[i * 128:(i + 1) * 128, :], reads=[wb_of.get("pv_b", wbuf0)], writes=[v_b])
                            vts.append((v_t, v_b))
                            pa, pab = aring.next()
                            for dc in range(32):
                                k.op("pe", lambda: nc.tensor.matmul(pa[:, 0:TG], lhsT=u_t[:, dc, :], rhs=AT[:, dc, :], start=(dc == 0), stop=(dc == 31)),
                                     reads=[u_b, ATb], writes=[pab], mark=(dc == 31))
                            ge_t, ge_b = ge.next()
                            k.op("act", lambda: nc.scalar.activation(out=ge_t[:], in_=pa[:, 0:TG], func=AF.Gelu), reads=[pab], writes=[ge_b])
                            k.op("dve", lambda: V.tensor_tensor(out=w_t[:, e_, :], in0=ge_t[:], in1=g_t[:].rearrange("p t n -> p (t n)"), op=ALU.mult),
                                 reads=[ge_b, g_b], writes=[w_b])
                        for js in range(4):
                            for db in range(8):
                                po, pob = oring.next()
                                for e_ in range(EG):
                                    k.op("pe", lambda: nc.tensor.matmul(po[:, 0:512], lhsT=w_t[:, e_, js * 128:(js + 1) * 128],
                                                                        rhs=vts[e_][0][:, db * 512:(db + 1) * 512], start=(e_ == 0), stop=(e_ == EG - 1)),
                                         reads=[w_b, vts[e_][1]], writes=[pob], mark=(e_ == EG - 1))
                                dst = acc[:, js, db * 512:(db + 1) * 512]
                                if eg == 0:
                                    k.op("dve", lambda: V.tensor_copy(out=dst, in_=po[:, 0:512]), reads=[pob], writes=[accb[js]])
                                else:
                                    k.op("dve", lambda: V.tensor_tensor(out=dst, in0=po[:, 0:512], in1=dst, op=ALU.add), reads=[pob, accb[js]], writes=[accb[js]])
                    for js in range(4):
                        tok0 = row0 + js * 128
                        xt, xb = xt_ring.next()
                        k.dma("sp", out=xt[:], in_=x1[tok0:tok0 + 128, :], writes=[xb])
                        for cc in range(4):
                            gc, gcb = gch.next()
                            k.dma("sp", out=gc[:], in_=mods[sset, 5 * D + cc * 1024:5 * D + (cc + 1) * 1024].partition_broadcast(128),
                                  reads=[modb], writes=[gcb])
                            a_ = acc[:, js, cc * 1024:(cc + 1) * 1024]
                            k.op("dve", lambda: V.tensor_tensor(out=a_, in0=a_, in1=gc[:], op=ALU.mult), reads=[accb[js], gcb], writes=[accb[js]])
                            k.op("dve", lambda: V.tensor_tensor(out=a_, in0=a_, in1=xt[:, cc * 1024:(cc + 1) * 1024], op=ALU.add),
                                 reads=[accb[js], xb], writes=[accb[js]])
                        sst, sb_ = small.next()
                        k.op("act", lambda: nc.scalar.activation(out=xn[:], in_=acc[:, js, :], func=AF.Square, accum_out=sst[:, 0:1]),
                             reads=[accb[js]], writes=[xnb, sb_])
                        ssb[0] = sb_
                        rs, rb = rstd_of(sst[:, 0:1], 1, D, small)
                        for cc in range(4):
                            gc, gcb = gch.next()
                            k.dma("sp", out=gc[:], in_=g_final[cc * 1024:(cc + 1) * 1024].partition_broadcast(128), writes=[gcb])
                            a_ = acc[:, js, cc * 1024:(cc + 1) * 1024]
                            k.op("dve", lambda: V.scalar_tensor_tensor(out=a_, in0=a_, scalar=rs[:, 0:1], in1=gc[:], op0=ALU.mult, op1=ALU.mult),
                                 reads=[accb[js], rb, gcb], writes=[accb[js]])
                        k.dma("pool", out=y[tok0:tok0 + 128, :], in_=acc[:, js, :], reads=[accb[js]])
                k.barrier()

        k.barrier()
    return nc


def _host_rope(TS):
    def tab(dim):
        n_rows = TS // 64
        rows = np.broadcast_to(np.arange(n_rows)[:, None], (n_rows, 64)).reshape(-1).astype(np.float32)
        cols = np.broadcast_to(np.arange(64)[None, :], (n_rows, 64)).reshape(-1).astype(np.float32)
        n_freq = dim // 4
        freqs = (np.float32(10000.0) ** (-np.arange(n_freq, dtype=np.float32) / np.float32(n_freq))).astype(np.float32)
        ang = np.concatenate([rows[:, None] * freqs, cols[:, None] * freqs], axis=-1).astype(np.float32)
        return np.stack([np.cos(ang), np.sin(ang)], axis=1).astype(np.float32)
    return tab(64), tab(128)


def make_in_maps(inp, cfg, n_cores):
    TS, TC, TP, NPB = cfg["TS"], cfg["TC"], cfg["TP"], cfg["NPB"]
    rm, rg = _host_rope(TS)
    f = lambda a: np.ascontiguousarray(np.asarray(a, dtype=np.float32))
    shared = {
        "w_mod": f(inp["w_mod"][0]), "b_mod": f(inp["b_mod"][0]), "g_norm1": f(inp["g_norm1"][0]),
        "g_norm2": f(inp["g_norm2"][0]), "w_in": f(inp["w_in"][0]), "g_q_a": f(inp["g_q_a"][0]),
        "w_q_b": f(inp["w_q_b"][0]), "g_kv_a": f(inp["g_kv_a"][0]), "w_kv_b": f(inp["w_kv_b"][0]),
        "g_gqa_q": f(inp["g_gqa_q"][0]), "g_gqa_k": f(inp["g_gqa_k"][0]), "w_mla_o": f(inp["w_mla_o"][0]),
        "w_gqa_o": f(inp["w_gqa_o"][0]), "w_out": f(inp["w_out"][0]), "w_peer_q": f(inp["w_peer_q"][0]),
        "sub_keys": f(inp["peer_sub_keys"][0]), "peer_u": f(inp["peer_u"][0]), "peer_v": f(inp["peer_v"][0]),
        "g_final": f(inp["g_final"]), "rope_m": rm, "rope_g": rg,
    }
    maps = []
    for b in range(n_cores):
        m = dict(shared)
        xp = np.asarray(inp["x_prompt"][NPB * b:NPB * (b + 1)], dtype=np.float32).reshape(NPB * TP, D)
        m["xin"] = np.ascontiguousarray(np.concatenate([np.asarray(inp["x_sample"][b], dtype=np.float32), xp], axis=0))
        m["cvec"] = np.ascontiguousarray(np.stack([np.asarray(inp["c"][b], dtype=np.float32),
                                                   np.asarray(inp["c_ctx"], dtype=np.float32)], axis=0))
        m["c_ckv"] = f(inp["cache_mla_ckv"][b, 0])
        m["c_kr"] = f(inp["cache_mla_krope"][b, 0])
        m["c_gk"] = f(inp["cache_gqa_k"][b, 0]).reshape(TC, 512)
        m["c_gv"] = f(inp["cache_gqa_v"][b, 0]).reshape(TC, 512)
        maps.append(m)
    return maps


FULL_CFG = dict(TS=4096, TC=512, TP=256, NPB=2)


def kernel(**inp):
    cfg = FULL_CFG
    n = 8
    nc = build(cfg)
    maps = make_in_maps(inp, cfg, n)
    res = run_bass_kernel_spmd(nc, maps, core_ids=list(range(n)))
    TS, TP, NPB = cfg["TS"], cfg["TP"], cfg["NPB"]
    R = res.results
    y_s = np.stack([R[b]["y"][:TS] for b in range(n)], axis=0)
    y_p = np.concatenate([R[b]["y"][TS:].reshape(NPB, TP, D) for b in range(n)], axis=0)
    ckv = np.concatenate([R[b]["n_ckv"].reshape(NPB, 1, TP, 512) for b in range(n)], axis=0)
    kr = np.concatenate([R[b]["n_kr"].reshape(NPB, 1, TP, 64) for b in range(n)], axis=0)
    gk = np.concatenate([R[b]["n_gk"].reshape(NPB, 1, TP, 4, 128) for b in range(n)], axis=0)
    gv = np.concatenate([R[b]["n_gv"].reshape(NPB, 1, TP, 4, 128) for b in range(n)], axis=0)
    return (y_p.astype(np.float32), y_s.astype(np.float32), ckv.astype(np.float32), kr.astype(np.float32),
            gk.astype(np.float32), gv.astype(np.float32))
```

```python
import numpy as np
from contextlib import ExitStack
import concourse.bass as bass
import concourse.mybir as mybir
from concourse.bass_utils import run_bass_kernel_spmd

F32 = mybir.dt.float32
BF16 = mybir.dt.bfloat16
U32 = mybir.dt.uint32
AF = mybir.ActivationFunctionType
ALU = mybir.AluOpType
AX = mybir.AxisListType

D = 4096
EPS = 1e-6
NDS = 48
NEG = -1.0e30


class Buf:
    __slots__ = ("w", "r", "multi")

    def __init__(self, multi=False):
        self.w = {}
        self.r = {}
        self.multi = multi


class KB:
    def __init__(self, nc, es):
        self.nc = nc
        self.E = {"pe": nc.tensor, "act": nc.scalar, "dve": nc.vector, "pool": nc.gpsimd, "sp": nc.sync}
        self.sem = {e: es.enter_context(nc.semaphore("sem_" + e)) for e in ("pe", "act", "dve", "pool")}
        self.cnt = {e: 0 for e in self.sem}
        self.seen = {e: {} for e in self.E}
        self.dsems = [es.enter_context(nc.semaphore("dsem%d" % i)) for i in range(NDS)]
        self.dval = [0] * NDS
        self.dnext = {"sp": 0, "pool": 0, "act": 0}
        self.drange = {"sp": (0, 32), "pool": (32, NDS), "act": (0, 32)}

    def _wait(self, eng, key, val):
        seen = self.seen[eng]
        if seen.get(key, 0) >= val:
            return
        if key[0] == "e":
            if key[1] == "pe":
                assert val <= self.cnt["pe"], "wait on unmarked PE instruction"
            sem = self.sem[key[1]]
        else:
            sem = self.dsems[key[1]]
        self.E[eng].wait_ge(sem, val)
        seen[key] = val

    def _deps(self, eng, reads, writes):
        need = {}
        for b in reads:
            for kk, v in b.w.items():
                if need.get(kk, 0) < v:
                    need[kk] = v
        for b in writes:
            if not b.multi:
                for kk, v in b.w.items():
                    if need.get(kk, 0) < v:
                        need[kk] = v
            for kk, v in b.r.items():
                if need.get(kk, 0) < v:
                    need[kk] = v
        for kk, v in need.items():
            if eng == "pe" and kk == ("e", "pe"):
                continue
            self._wait(eng, kk, v)

    def _post(self, key, val, reads, writes):
        for b in reads:
            if b.r.get(key, 0) < val:
                b.r[key] = val
        for b in writes:
            if b.multi:
                if b.w.get(key, 0) < val:
                    b.w[key] = val
            else:
                b.w = {key: val}
                b.r = {}

    def op(self, eng, fn, reads=(), writes=(), mark=True):
        self._deps(eng, reads, writes)
        ins = fn()
        val = self.cnt[eng] + 1
        if mark:
            ins.then_inc(self.sem[eng], 1)
            self.cnt[eng] = val
        self._post(("e", eng), val, reads, writes)

    def dma(self, q, out, in_, reads=(), writes=(), **kw):
        self._deps(q, reads, writes)
        lo, hi = self.drange[q]
        slot = lo + self.dnext[q]
        self.dnext[q] = (self.dnext[q] + 1) % (hi - lo)
        if self.dval[slot] > 0:
            self._wait(q, ("d", slot), self.dval[slot])
        ins = self.E[q].dma_start(out=out, in_=in_, **kw)
        self.dval[slot] += 16
        ins.then_inc(self.dsems[slot], 16)
        self._post(("d", slot), self.dval[slot], reads, writes)

    def barrier(self):
        for eng in self.E:
            for e in self.sem:
                if self.cnt[e] > 0:
                    self._wait(eng, ("e", e), self.cnt[e])
            for s in range(NDS):
                if self.dval[s] > 0:
                    self._wait(eng, ("d", s), self.dval[s])


class Ring:
    def __init__(self, tiles, multi=False):
        self.tiles = tiles
        self.bufs = [Buf(multi) for _ in tiles]
        self.i = 0

    def next(self):
        t, b = self.tiles[self.i], self.bufs[self.i]
        self.i = (self.i + 1) % len(self.tiles)
        return t, b


def build(cfg):
    TS, TC, TP, NPB = cfg["TS"], cfg["TC"], cfg["TP"], cfg["NPB"]
    debug = cfg.get("debug", ())
    stop_after = cfg.get("stop_after", 9)
    NQ = TS + NPB * TP
    NK = TS + TC + NPB * TP
    TG = 512
    assert TS % TG == 0 and TC % TG == 0 and NPB * TP == TG
    NTG = NQ // TG
    NKG = NK // TG
    NKT = NK // 128
    NQT = NQ // 128
    seqs = [(0, TS, 0, TS + TC)]
    for i in range(NPB):
        seqs.append((TS + i * TP, TP, TS + TC + i * TP, TP))

    nc = bass.Bass("TRN2", target_bir_lowering=False)

    only = cfg.get("only")
    dins = []

    def din(name, shape, dt=F32):
        if only and name not in ("sub_keys",):
            shape = [2, 2]
        dins.append((name, list(shape)))
        return nc.dram_tensor(name, list(shape), dt, kind="ExternalInput").ap()

    def dout(name, shape, dt=F32):
        return nc.dram_tensor(name, list(shape), dt, kind="ExternalOutput").ap()

    def dscr(name, shape, dt=BF16):
        kind = "ExternalOutput" if name in debug else "Internal"
        return nc.dram_tensor(name, list(shape), dt, kind=kind).ap()

    xin = din("xin", [NQ, D])
    cvec = din("cvec", [2, D])
    c_ckv = din("c_ckv", [TC, 512])
    c_kr = din("c_kr", [TC, 64])
    c_gk = din("c_gk", [TC, 512])
    c_gv = din("c_gv", [TC, 512])
    w_mod = din("w_mod", [D, 6 * D])
    b_mod = din("b_mod", [6 * D])
    g_norm1 = din("g_norm1", [D])
    g_norm2 = din("g_norm2", [D])
    w_in = din("w_in", [D, 12864])
    g_q_a = din("g_q_a", [1024])
    w_q_b = din("w_q_b", [1024, 3072])
    g_kv_a = din("g_kv_a", [512])
    w_kv_b = din("w_kv_b", [512, 4096])
    g_gqa_q = din("g_gqa_q", [128])
    g_gqa_k = din("g_gqa_k", [128])
    w_mla_o = din("w_mla_o", [2048, D])
    w_gqa_o = din("w_gqa_o", [2048, D])
    w_out = din("w_out", [D, D])
    w_peer_q = din("w_peer_q", [D, 2048])
    sub_keys = din("sub_keys", [2, 128, 128])
    peer_u = din("peer_u", [16384, D])
    peer_v = din("peer_v", [16384, D])
    g_final = din("g_final", [D])
    rope_m = din("rope_m", [TS, 2, 32])
    rope_g = din("rope_g", [TS, 2, 64])

    y = dout("y", [NQ, D])
    n_ckv = dout("n_ckv", [NPB * TP, 512])
    n_kr = dout("n_kr", [NPB * TP, 64])
    n_gk = dout("n_gk", [NPB * TP, 512])
    n_gv = dout("n_gv", [NPB * TP, 512])

    w_in_b = dscr("w_in_b", [D, 12864])
    w_q_bb = dscr("w_q_bb", [1024, 3072])
    w_kv_bb = dscr("w_kv_bb", [512, 4096])
    w_mla_ob = dscr("w_mla_ob", [2048, D])
    w_gqa_ob = dscr("w_gqa_ob", [2048, D])
    w_out_b = dscr("w_out_b", [D, D])
    w_pq_b = dscr("w_pq_b", [D, 2048])
    pv_b = dscr("pv_b", [16384, D])
    ut_b = dscr("ut_b", [128, 128, 32, 128])
    mods = dscr("mods", [2, 6 * D], F32)
    qan = dscr("qan", [NQ, 1024])
    ckv_s = dscr("ckv_s", [NK, 512])
    qTn = dscr("qTn", [16, 128, NQ])
    qTr = dscr("qTr", [16, 64, NQ])
    kTn = dscr("kTn", [16, 128, NK])
    kTr = dscr("kTr", [64, NK])
    vm = dscr("vm", [16, 128, NKT, 128])
    gqT = dscr("gqT", [16, 128, NQ])
    gkT = dscr("gkT", [4, 128, NK])
    gvm = dscr("gvm", [4, 128, NKT, 128])
    o_mla = dscr("o_mla", [NQ, 2048])
    o_gqa = dscr("o_gqa", [NQ, 2048])
    merged = dscr("merged", [NQ, D])
    x1 = dscr("x1", [NQ, D], F32)
    Gs = dscr("Gs", [NQT, 128, 128, 128])
    if only == "d1b":
        qpT = nc.dram_tensor("qpT", [16, 128, NQ], BF16, kind="ExternalInput").ap()
    else:
        qpT = dscr("qpT", [16, 128, NQ])

    es = ExitStack()
    with es:
        k = KB(nc, es)

        def sb(st, name, shape, dt):
            return st.enter_context(nc.sbuf_tensor(name, list(shape), dt))

        pf = [es.enter_context(nc.psum_tensor("pf%d" % i, [128, 512], F32)) for i in range(6)]
        pfb = [Buf() for _ in range(6)]
        ptb = [es.enter_context(nc.psum_tensor("ptb%d" % i, [128, 1024], BF16)) for i in range(2)]
        pT = Ring(ptb)

        class PRing:
            def __init__(self, idx):
                self.idx = idx
                self.i = 0

            def next(self):
                j = self.idx[self.i]
                self.i = (self.i + 1) % len(self.idx)
                return pf[j], pfb[j]

        ident_f = sb(es, "ident_f", [128, 128], F32)
        ident = sb(es, "ident", [128, 128], BF16)
        iota_f = sb(es, "iota_f", [128, 128], F32)
        cb_ = Buf()
        k.op("pool", lambda: nc.gpsimd.iota(ident_f[:], pattern=[[1, 128]], base=0, channel_multiplier=-1,
                                            allow_small_or_imprecise_dtypes=True), writes=[cb_])
        k.op("pool", lambda: nc.gpsimd.iota(iota_f[:], pattern=[[1, 128]], base=0, channel_multiplier=0,
                                            allow_small_or_imprecise_dtypes=True), writes=[cb_])
        k.op("dve", lambda: nc.vector.tensor_single_scalar(out=ident[:], in_=ident_f[:], scalar=0.0, op=ALU.is_equal),
             reads=[cb_], writes=[cb_])
        k.op("dve", lambda: nc.vector.tensor_single_scalar(out=ident_f[:], in_=ident_f[:], scalar=0.0, op=ALU.is_equal),
             reads=[cb_], writes=[cb_])
        fm = sb(es, "fm", [128, 2, 4, 32], F32)
        gn = sb(es, "gn", [128, 2, 32], F32)
        Gm = sb(es, "Gm", [128, 2, 2, 32], F32)
        modb = Buf()

        wbuf0 = Buf(multi=True)

        wb_of = {}
        deferred = []

        def cast_copy(dst, src, rows, rchunk, c0=0, c1=None, defer=False):
            if only:
                return
            b = wb_of.setdefault(dst.name, Buf(multi=True))
            for r0 in range(0, rows, rchunk):
                if c1 is None:
                    o_, i_ = dst[r0:r0 + rchunk, :], src[r0:r0 + rchunk, :]
                else:
                    o_, i_ = dst[r0:r0 + rchunk, c0:c1], src[r0:r0 + rchunk, c0:c1]
                fn = (lambda o_=o_, i_=i_, b=b: k.dma("pool", out=o_, in_=i_, writes=[b], max_dma_last_dim=8192))
                if defer:
                    deferred.append(fn)
                else:
                    fn()

        def flush_casts(n):
            for _ in range(min(n, len(deferred))):
                deferred.pop(0)()

        if only:
            stop_after = -1
        cast_copy(w_in_b, w_in, D, 512, 0, 4672)
        cast_copy(w_q_bb, w_q_b, 1024, 512, defer=True)
        cast_copy(w_kv_bb, w_kv_b, 512, 512, defer=True)
        cast_copy(ckv_s[TS:TS + TC, :], c_ckv, TC, 512, defer=True)
        if stop_after > 3:
            cast_copy(w_mla_ob, w_mla_o, 2048, 512, defer=True)
            cast_copy(w_gqa_ob, w_gqa_o, 2048, 512, defer=True)
            cast_copy(w_in_b, w_in, D, 512, 4672, 12864, defer=True)
            cast_copy(w_out_b, w_out, D, 512, defer=True)
        if stop_after > 5:
            cast_copy(w_pq_b, w_peer_q, D, 1024, defer=True)
            cast_copy(pv_b, peer_v, 16384, 1024, defer=True)
        if stop_after >= 1:
            with ExitStack() as ps:
                cT = sb(ps, "cT", [128, 2, 32], F32)
                sc = sb(ps, "sc", [128, 2, 32], BF16)
                cTb = Buf()
                wm = Ring([sb(ps, "wm%d" % i, [128, 32, 512], BF16) for i in range(2)])
                bb = Ring([sb(ps, "bb%d" % i, [2, 512], F32) for i in range(2)])
                mst = Ring([sb(ps, "mst%d" % i, [2, 512], F32) for i in range(2)])
                mring = PRing([0, 1])
                Rv = sb(ps, "Rv", [32, 12, 128], F32)
                Rvb = Buf()
                k.dma("sp", out=Rv[:, 0:2, :], in_=cvec.rearrange("s (kc p) -> kc s p", p=128), writes=[Rvb])
                ptv, pbv = mring.next()
                for v in range(2):
                    k.op("pe", lambda: nc.tensor.transpose(ptv[:, v * 32:(v + 1) * 32], Rv[0:32, v, :], ident_f[0:32, 0:32]),
                         reads=[Rvb, cb_], writes=[pbv], mark=(v == 1))
                k.op("dve", lambda: nc.vector.tensor_copy(out=cT[:], in_=ptv[:, 0:64].rearrange("p (s k) -> p s k", k=32)),
                     reads=[pbv], writes=[cTb])
                k.op("act", lambda: nc.scalar.activation(out=sc[:], in_=cT[:], func=AF.Silu), reads=[cTb], writes=[cTb])
                wmv = w_mod.rearrange("(kc p) n -> p kc n", p=128)
                for c in range(48):
                    wt, wb = wm.next()
                    k.dma("pool", out=wt[:], in_=wmv[:, :, c * 512:(c + 1) * 512], writes=[wb])
                    bt, bbf = bb.next()
                    k.dma("sp", out=bt[:], in_=b_mod[c * 512:(c + 1) * 512].partition_broadcast(2), writes=[bbf])
                    pt, pb = mring.next()
                    for kc in range(32):
                        k.op("pe", lambda: nc.tensor.matmul(pt[0:2, :], lhsT=sc[:, :, kc], rhs=wt[:, kc, :],
                                                            start=(kc == 0), stop=(kc == 31)),
                             reads=[cTb, wb], writes=[pb], mark=(kc == 31))
                    mt, mb = mst.next()
                    k.op("dve", lambda: nc.vector.tensor_tensor(out=mt[:], in0=pt[0:2, :], in1=bt[:], op=ALU.add),
                         reads=[pb, bbf], writes=[mb])
                    k.dma("sp", out=mods[:, c * 512:(c + 1) * 512], in_=mt[:], reads=[mb], writes=[modb])
                mv = mods.rearrange("s (v kc p) -> kc s v p", p=128, kc=32)
                vi = 0
                for s in range(2):
                    for a, v in enumerate((1, 0, 4, 3)):
                        k.dma("sp", out=Rv[:, vi, :], in_=mv[:, s, v, :], reads=[modb, Rvb], writes=[Rvb])
                        vi += 1
                k.dma("sp", out=Rv[:, 8, :], in_=g_norm1.rearrange("(kc p) -> kc p", p=128), writes=[Rvb])
                k.dma("sp", out=Rv[:, 9, :], in_=g_norm2.rearrange("(kc p) -> kc p", p=128), writes=[Rvb])
                ptv, pbv = mring.next()
                for v in range(10):
                    k.op("pe", lambda: nc.tensor.transpose(ptv[:, v * 32:(v + 1) * 32], Rv[0:32, v, :], ident_f[0:32, 0:32]),
                         reads=[Rvb, cb_], writes=[pbv], mark=(v == 9))
                k.op("dve", lambda: nc.vector.tensor_copy(out=fm[:].rearrange("p s a k -> p (s a k)"), in_=ptv[:, 0:256]),
                     reads=[pbv], writes=[cb_])
                k.op("dve", lambda: nc.vector.tensor_copy(out=gn[:].rearrange("p a k -> p (a k)"), in_=ptv[:, 256:320]),
                     reads=[pbv], writes=[cb_])
                for s in range(2):
                    for a in range(2):
                        k.op("dve", lambda: nc.vector.scalar_tensor_tensor(
                            out=Gm[:, s, a, :], in0=fm[:, s, 2 * a, :], scalar=1.0, in1=gn[:, a, :],
                            op0=ALU.add, op1=ALU.mult), reads=[cb_], writes=[cb_])
                k.barrier()

        evac_rr = [0]

        def evac_copy(out, in_, reads, writes):
            evac_rr[0] ^= 1
            if evac_rr[0]:
                k.op("act", lambda: nc.scalar.copy(out=out, in_=in_), reads=reads, writes=writes)
            else:
                k.op("dve", lambda: nc.vector.tensor_copy(out=out, in_=in_), reads=reads, writes=writes)

        def transpose_blocks(blocks, dst_fn, src_bufs, dst_buf):
            i = 0
            while i < len(blocks):
                n = min(8, len(blocks) - i)
                pt, pb = pT.next()
                w = blocks[i].shape[1]
                for t in range(n):
                    blk = blocks[i + t]
                    k.op("pe", lambda: nc.tensor.transpose(pt[0:w, t * 128:(t + 1) * 128], blk, ident[:]),
                         reads=list(src_bufs) + [cb_], writes=[pb], mark=(t == n - 1))
                evac_copy(dst_fn(i, n, w), pt[0:w, 0:n * 128].rearrange("p (n t) -> p n t", t=128), [pb], [dst_buf])
                i += n

        def rstd_of(ss, nh, hd, small):
            t1, b1 = small.next()
            k.op("dve", lambda: nc.vector.tensor_scalar(out=t1[:, :nh], in0=ss, scalar1=1.0 / hd, scalar2=EPS,
                                                        op0=ALU.mult, op1=ALU.add), reads=[ssb[0]], writes=[b1])
            k.op("act", lambda: nc.scalar.activation(out=t1[:, :nh], in_=t1[:, :nh], func=AF.Sqrt),
                 reads=[b1], writes=[b1])
            k.op("dve", lambda: nc.vector.reciprocal(out=t1[:, :nh], in_=t1[:, :nh]), reads=[b1], writes=[b1])
            return t1[:, :nh], b1

        ssb = [None]

        def load_hT(st, AT, ATb, src, row0, which, bufs):
            xt_ring, xn, xnb, small = bufs
            s = 0 if row0 < TS else 1
            for j in range(TG // 128):
                xt, xb = xt_ring.next()
                k.dma("sp", out=xt[:], in_=src[row0 + j * 128: row0 + (j + 1) * 128, :], writes=[xb])
                sst, sb_ = small.next()
                k.op("act", lambda: nc.scalar.activation(out=xn[:], in_=xt[:], func=AF.Square, accum_out=sst[:, 0:1]),
                     reads=[xb], writes=[xnb, sb_])
                ssb[0] = sb_
                rs, rb = rstd_of(sst[:, 0:1], 1, D, small)
                k.op("dve", lambda: nc.vector.tensor_scalar(out=xn[:], in0=xt[:], scalar1=rs[:, 0:1], scalar2=None,
                                                            op0=ALU.mult), reads=[xb, rb], writes=[xnb])
                for g in range(4):
                    pt, pb = pT.next()
                    for t in range(8):
                        kc = g * 8 + t
                        k.op("pe", lambda: nc.tensor.transpose(pt[:, t * 128:(t + 1) * 128],
                                                               xn[:, kc * 128:(kc + 1) * 128], ident[:]),
                             reads=[xnb, cb_], writes=[pb], mark=(t == 7))
                    for t in range(8):
                        kc = g * 8 + t
                        o = AT[:, kc, j * 128:(j + 1) * 128]
                        i_ = pt[:, t * 128:(t + 1) * 128]
                        sc_ = Gm[:, s, which, kc:kc + 1]
                        bi_ = fm[:, s, 2 * which + 1, kc:kc + 1]
                        if True:
                            k.op("act", lambda: nc.scalar.activation(out=o, in_=i_, func=AF.Identity, bias=bi_, scale=sc_),
                                 reads=[pb, cb_], writes=[ATb])
                        else:
                            k.op("dve", lambda: nc.vector.tensor_scalar(out=o, in0=i_, scalar1=sc_, scalar2=bi_,
                                                                        op0=ALU.mult, op1=ALU.add),
                                 reads=[pb, cb_], writes=[ATb])

        def load_AT(AT, ATb, src, row0, K, a_ring, col0=0):
            for j in range(TG // 128):
                at, ab = a_ring.next()
                k.dma("sp", out=at[:, :K], in_=src[row0 + j * 128: row0 + (j + 1) * 128, col0:col0 + K], writes=[ab])
                blocks = [at[:, kc * 128:(kc + 1) * 128] for kc in range(K // 128)]
                transpose_blocks(blocks, lambda i, n, w: AT[:, i:i + n, j * 128:(j + 1) * 128], [ab], ATb)

        def wload(wr, src, Kc, width):
            wt, wb = wr.next()
            wbuf = wb_of.get(src.name, wbuf0)
            if len(src.shape) == 2:
                src = src.rearrange("(kc p) n -> p kc n", p=128)
                k.dma("sp", out=wt[:, :Kc, :width], in_=src, reads=[wbuf], writes=[wb])
            else:
                a, b = src.shape[1], src.shape[2]
                for ai in range(a):
                    k.dma("sp", out=wt[:, :Kc, ai * b:(ai + 1) * b],
                          in_=src[:, ai, :].rearrange("(kc p) b -> p kc b", p=128), reads=[wbuf], writes=[wb])
            return wt, wb

        def gemm_run(AT, ATb, Kc, wt, wb, width, orient, pring, epi):
            for j in range(4):
                pt, pb = pring.next()
                for kc in range(Kc):
                    if orient == "A":
                        k.op("pe", lambda: nc.tensor.matmul(pt[:, :width], lhsT=AT[:, kc, j * 128:(j + 1) * 128],
                                                            rhs=wt[:, kc, :width], start=(kc == 0), stop=(kc == Kc - 1)),
                             reads=[ATb, wb], writes=[pb], mark=(kc == Kc - 1))
                    else:
                        k.op("pe", lambda: nc.tensor.matmul(pt[:, :TG], lhsT=wt[:, kc, j * 128:(j + 1) * 128],
                                                            rhs=AT[:, kc, :], start=(kc == 0), stop=(kc == Kc - 1)),
                             reads=[ATb, wb], writes=[pb], mark=(kc == Kc - 1))
                epi(j, pt, pb)

        def gemm_list(AT, ATb, Kc, jobs, wr, pring):
            def kc_of(job):
                return job[6] if len(job) > 4 else Kc
            nxt = wload(wr, jobs[0][0], kc_of(jobs[0]), jobs[0][1])
            for i, job in enumerate(jobs):
                src, width, orient, epi = job[:4]
                cur = nxt
                if i + 1 < len(jobs):
                    nxt = wload(wr, jobs[i + 1][0], kc_of(jobs[i + 1]), jobs[i + 1][1])
                if len(job) > 4:
                    gemm_run(job[4], job[5], job[6], cur[0], cur[1], width, orient, pring, epi)
                else:
                    gemm_run(AT, ATb, Kc, cur[0], cur[1], width, orient, pring, epi)

        def rmsnorm_heads(src3, srcbufs, nh, hd, g_bc, dst3, dstb, sq, sqb, small):
            k.op("act", lambda: nc.scalar.activation(out=sq[:, :nh * hd].rearrange("p (h d) -> p h d", d=hd), in_=src3,
                                                     func=AF.Square), reads=srcbufs, writes=[sqb])
            sst, sb_ = small.next()
            k.op("dve", lambda: nc.vector.tensor_reduce(out=sst[:, :nh], in_=sq[:, :nh * hd].rearrange("p (h d) -> p h d", d=hd),
                                                        axis=AX.X, op=ALU.add), reads=[sqb], writes=[sb_])
            ssb[0] = sb_
            rs, rb = rstd_of(sst[:, :nh], nh, hd, small)
            k.op("dve", lambda: nc.vector.tensor_tensor(out=dst3, in0=src3, in1=rs.unsqueeze(2).broadcast_to([128, nh, hd]),
                                                        op=ALU.mult), reads=list(srcbufs) + [rb], writes=[dstb])
            k.op("dve", lambda: nc.vector.tensor_tensor(out=dst3, in0=dst3, in1=g_bc.unsqueeze(1).broadcast_to([128, nh, hd]),
                                                        op=ALU.mult), reads=[dstb, cb_], writes=[dstb])

        def rope(t3, tb, nh, hd, cs, csb, tmp, tmpb):
            h2 = hd // 2
            v = t3.rearrange("p h (i two) -> p h i two", two=2)
            x1_, x2_ = v[:, :, :, 0], v[:, :, :, 1]
            cosb = cs[:, 0, :].unsqueeze(1).broadcast_to([128, nh, h2])
            sinb = cs[:, 1, :].unsqueeze(1).broadcast_to([128, nh, h2])
            ta = tmp[:, 0:nh * h2].rearrange("p (h i) -> p h i", i=h2)
            tb_ = tmp[:, nh * h2:2 * nh * h2].rearrange("p (h i) -> p h i", i=h2)
            tc_ = tmp[:, 2 * nh * h2:3 * nh * h2].rearrange("p (h i) -> p h i", i=h2)
            V = nc.vector
            k.op("dve", lambda: V.tensor_tensor(out=ta, in0=x1_, in1=sinb, op=ALU.mult), reads=[tb, csb], writes=[tmpb])
            k.op("dve", lambda: V.tensor_tensor(out=tb_, in0=x2_, in1=sinb, op=ALU.mult), reads=[tb, csb], writes=[tmpb])
            k.op("dve", lambda: V.tensor_tensor(out=x1_, in0=x1_, in1=cosb, op=ALU.mult), reads=[tb, csb], writes=[tb])
            k.op("dve", lambda: V.tensor_tensor(out=tc_, in0=x2_, in1=cosb, op=ALU.mult), reads=[tb, csb], writes=[tmpb])
            k.op("dve", lambda: V.tensor_tensor(out=x1_, in0=x1_, in1=tb_, op=ALU.subtract), reads=[tb, tmpb], writes=[tb])
            k.op("dve", lambda: V.tensor_tensor(out=x2_, in0=ta, in1=tc_, op=ALU.add), reads=[tmpb], writes=[tb])

        scrb = Buf(multi=True)
        if stop_after >= 2:
            with ExitStack() as ps:
                AT = sb(ps, "AT", [128, 32, TG], BF16)
                ATb = Buf(multi=True)
                xt_ring = Ring([sb(ps, "xt%d" % i, [128, D], F32) for i in range(2)])
                xn = sb(ps, "xn", [128, D], BF16)
                xnb = Buf()
                small = Ring([sb(ps, "sm%d" % i, [128, 8], F32) for i in range(8)])
                wr = Ring([sb(ps, "wr%d" % i, [128, 32, 512], BF16) for i in range(2)])
                qa_st = sb(ps, "qa_st", [128, 4, 1024], F32)
                qab = [Buf(multi=True) for _ in range(4)]
                st32 = Ring([sb(ps, "st32_%d" % i, [128, 1024], F32) for i in range(2)])
                sq = sb(ps, "sq", [128, 1024], F32)
                sqb = Buf()
                tmp = sb(ps, "tmp", [128, 768], F32)
                tmpb = Buf()
                st16 = Ring([sb(ps, "st16_%d" % i, [128, 1024], BF16) for i in range(3)])
                gT = Ring([sb(ps, "gT%d" % i, [128, 4, TG], BF16) for i in range(2)])
                gqa_bc = sb(ps, "gqa_bc", [128, 1024], F32)
                gkv_bc = sb(ps, "gkv_bc", [128, 512], F32)
                ggq_bc = sb(ps, "ggq_bc", [128, 128], F32)
                ggk_bc = sb(ps, "ggk_bc", [128, 128], F32)
                rm = sb(ps, "rm", [128, 4, 2, 32], F32)
                rg = sb(ps, "rg", [128, 4, 2, 64], F32)
                rb_ = Buf(multi=True)
                k.dma("sp", out=gqa_bc[:], in_=g_q_a.partition_broadcast(128), writes=[cb_])
                k.dma("sp", out=gkv_bc[:], in_=g_kv_a.partition_broadcast(128), writes=[cb_])
                k.dma("sp", out=ggq_bc[:], in_=g_gqa_q.partition_broadcast(128), writes=[cb_])
                k.dma("sp", out=ggk_bc[:], in_=g_gqa_k.partition_broadcast(128), writes=[cb_])
                pring = PRing([0, 1, 2, 3])
                col_blocks = [(0, 512), (512, 512), (1024, 512), (1536, 64), (1600, 512), (2112, 512), (2624, 512),
                              (3136, 512), (3648, 512), (4160, 512)]

                def cache_side():
                    for t in range(TC // 128):
                        kt = (TS // 128) + t
                        r0 = t * 128
                        s32, s32b = st32.next()
                        s16, s16b = st16.next()
                        k.dma("sp", out=s32[:, 0:512], in_=c_gk[r0:r0 + 128, :], writes=[s32b])
                        k.dma("sp", out=s32[:, 512:576], in_=c_kr[r0:r0 + 128, :], writes=[s32b])
                        evac_copy(s16[:, 0:576], s32[:, 0:576], [s32b], [s16b])
                        g_t, g_b = gT.next()
                        transpose_blocks([s16[:, h * 128:(h + 1) * 128] for h in range(4)],
                                         lambda i, n, w: g_t[:, i:i + n, 0:128], [s16b], g_b)
                        k.dma("pool", out=gkT[:, :, TS + r0:TS + r0 + 128].rearrange("h d t -> d h t"), in_=g_t[:, :, 0:128],
                              reads=[g_b], writes=[scrb])
                        g_t, g_b = gT.next()
                        transpose_blocks([s16[:, 512:576]], lambda i, n, w: g_t[0:64, 0:1, 0:128], [s16b], g_b)
                        k.dma("pool", out=kTr[:, TS + r0:TS + r0 + 128], in_=g_t[0:64, 0, 0:128], reads=[g_b], writes=[scrb])
                        s32, s32b = st32.next()
                        s16, s16b = st16.next()
                        k.dma("sp", out=s32[:, 0:512], in_=c_gv[r0:r0 + 128, :], writes=[s32b])
                        evac_copy(s16[:, 0:512], s32[:, 0:512], [s32b], [s16b])
                        k.dma("pool", out=gvm[:, :, kt, :].rearrange("h p d -> p h d"),
                              in_=s16[:, 0:512].rearrange("p (h d) -> p h d", d=128), reads=[s16b], writes=[scrb])

                if cfg.get('cache', True):
                    cache_side()

                for g in range(NTG):
                    row0 = g * TG
                    sample = row0 < TS
                    krow0 = row0 if sample else row0 + TC
                    flush_casts(6 if g == 0 else 3)
                    load_hT(ps, AT, ATb, xin, row0, 0, (xt_ring, xn, xnb, small))
                    if sample:
                        k.dma("sp", out=rm[:], in_=rope_m[row0:row0 + TG].rearrange("(j p) a i -> p j a i", p=128), writes=[rb_])
                        k.dma("sp", out=rg[:], in_=rope_g[row0:row0 + TG].rearrange("(j p) a i -> p j a i", p=128), writes=[rb_])
                    tr_state = {}

                    def make_epi(c):
                        c0, width = col_blocks[c]

                        def epi(j, pt, pb):
                            tok0 = row0 + j * 128
                            ktok0 = krow0 + j * 128
                            kt = ktok0 // 128
                            prow = tok0 - TS
                            if c in (0, 1):
                                evac_copy(qa_st[:, j, c * 512:(c + 1) * 512], pt[:, :512], [pb], [qab[j]])
                                if c == 1:
                                    s16, s16b = st16.next()
                                    s32, s32b = st32.next()
                                    rmsnorm_heads(qa_st[:, j:j + 1, :], [qab[j]], 1, 1024, gqa_bc[:],
                                                  s32[:, 0:1024].rearrange("p (h d) -> p h d", h=1), s32b, sq, sqb, small)
                                    evac_copy(s16[:, 0:1024], s32[:, 0:1024], [s32b], [s16b])
                                    k.dma("pool", out=qan[tok0:tok0 + 128, :], in_=s16[:, 0:1024], reads=[s16b], writes=[scrb])
                            elif c == 2:
                                s32, s32b = st32.next()
                                s16, s16b = st16.next()
                                rmsnorm_heads(pt[:, 0:512].rearrange("p (h d) -> p h d", h=1), [pb], 1, 512, gkv_bc[:],
                                              s32[:, 0:512].rearrange("p (h d) -> p h d", h=1), s32b, sq, sqb, small)
                                if not sample:
                                    k.dma("pool", out=n_ckv[prow:prow + 128, :], in_=s32[:, 0:512], reads=[s32b])
                                evac_copy(s16[:, 0:512], s32[:, 0:512], [s32b], [s16b])
                                k.dma("pool", out=ckv_s[ktok0:ktok0 + 128, :], in_=s16[:, 0:512], reads=[s16b], writes=[scrb])
                            elif c == 3:
                                s32, s32b = st32.next()
                                s16, s16b = st16.next()
                                evac_copy(s32[:, 0:64], pt[:, 0:64], [pb], [s32b])
                                if sample:
                                    rope(s32[:, 0:64].rearrange("p (h d) -> p h d", h=1), s32b, 1, 64, rm[:, j], rb_, tmp, tmpb)
                                else:
                                    k.dma("pool", out=n_kr[prow:prow + 128, :], in_=s32[:, 0:64], reads=[s32b])
                                evac_copy(s16[:, 0:64], s32[:, 0:64], [s32b], [s16b])
                                if j == 0:
                                    tr_state["kr"] = gT.next()
                                g_t, g_b = tr_state["kr"]
                                transpose_blocks([s16[:, 0:64]], lambda i, n, w: g_t[0:64, 0:1, j * 128:(j + 1) * 128], [s16b], g_b)
                                if j == 3:
                                    k.dma("pool", out=kTr[:, krow0:krow0 + TG], in_=g_t[0:64, 0, :], reads=[g_b], writes=[scrb])
                            elif c in (4, 5, 6, 7, 8):
                                isq = c < 8
                                s32, s32b = st32.next()
                                s16, s16b = st16.next()
                                d3 = s32[:, 0:512].rearrange("p (h d) -> p h d", d=128)
                                rmsnorm_heads(pt[:, 0:512].rearrange("p (h d) -> p h d", d=128), [pb], 4, 128,
                                              ggq_bc[:] if isq else ggk_bc[:], d3, s32b, sq, sqb, small)
                                if sample:
                                    rope(d3, s32b, 4, 128, rg[:, j], rb_, tmp, tmpb)
                                elif not isq:
                                    k.dma("pool", out=n_gk[prow:prow + 128, :], in_=s32[:, 0:512], reads=[s32b])
                                evac_copy(s16[:, 0:512], s32[:, 0:512], [s32b], [s16b])
                                if j == 0:
                                    tr_state[c] = gT.next()
                                g_t, g_b = tr_state[c]
                                transpose_blocks([s16[:, h * 128:(h + 1) * 128] for h in range(4)],
                                                 lambda i, n, w: g_t[:, i:i + n, j * 128:(j + 1) * 128], [s16b], g_b)
                                if j == 3:
                                    if isq:
                                        h0 = (c - 4) * 4
                                        k.dma("pool", out=gqT[h0:h0 + 4, :, row0:row0 + TG].rearrange("h d t -> d h t"), in_=g_t[:],
                                              reads=[g_b], writes=[scrb])
                                    else:
                                        k.dma("pool", out=gkT[:, :, krow0:krow0 + TG].rearrange("h d t -> d h t"), in_=g_t[:],
                                              reads=[g_b], writes=[scrb])
                            else:
                                s32, s32b = st32.next()
                                s16, s16b = st16.next()
                                evac_copy(s32[:, 0:512], pt[:, 0:512], [pb], [s32b])
                                if not sample:
                                    k.dma("pool", out=n_gv[prow:prow + 128, :], in_=s32[:, 0:512], reads=[s32b])
                                evac_copy(s16[:, 0:512], s32[:, 0:512], [s32b], [s16b])
                                k.dma("pool", out=gvm[:, :, kt, :].rearrange("h p d -> p h d"),
                                      in_=s16[:, 0:512].rearrange("p (h d) -> p h d", d=128), reads=[s16b], writes=[scrb])
                        return epi

                    jobs = [(w_in_b[:, c0:c0 + wd], wd, "A", make_epi(c)) for c, (c0, wd) in enumerate(col_blocks) if c in cfg.get('ablk', range(10))]
                    if jobs:
                        gemm_list(AT, ATb, 32, jobs, wr, pring)
                k.barrier()

        pass

        if stop_after >= 3:
            with ExitStack() as ps:
                AT = sb(ps, "AT2", [128, 8, TG], BF16)
                ATb = Buf(multi=True)
                a_ring = Ring([sb(ps, "a2_%d" % i, [128, 1024], BF16) for i in range(2)])
                wr = Ring([sb(ps, "wr2_%d" % i, [128, 8, 512], BF16) for i in range(2)], multi=True)
                st16 = Ring([sb(ps, "s16_%d" % i, [128, 512], BF16) for i in range(3)])
                st32 = Ring([sb(ps, "s32_%d" % i, [128, 512], F32) for i in range(2)])
                tmp = sb(ps, "tmp2", [128, 768], F32)
                tmpb = Buf()
                qr_st = Ring([sb(ps, "qr_st%d" % i, [64, 8, TG], BF16) for i in range(2)])
                rm = sb(ps, "rm2", [128, 4, 2, 32], F32)
                rb_ = Buf(multi=True)
                pring = PRing([0, 1, 2, 3])
                wq3 = w_q_bb.rearrange("k (h c) -> k h c", c=192)
                wkv3 = w_kv_bb.rearrange("k (h c) -> k h c", c=256)
                for g in range(NTG):
                    row0 = g * TG
                    sample = row0 < TS
                    load_AT(AT, ATb, qan, row0, 1024, a_ring)
                    if sample:
                        k.dma("sp", out=rm[:], in_=rope_m[row0:row0 + TG].rearrange("(j p) a i -> p j a i", p=128), writes=[rb_])
                    jobs = []
                    for h0 in range(0, 16, 4):
                        def epi_n(j, pt, pb, h0=h0):
                            s16, s16b = st16.next()
                            evac_copy(s16[:, 0:TG], pt[:, 0:TG], [pb], [s16b])
                            k.dma("pool", out=qTn[h0 + j, :, row0:row0 + TG], in_=s16[:, 0:TG], reads=[s16b], writes=[scrb])
                        jobs.append((wq3[:, h0:h0 + 4, 0:128], 512, "B", epi_n))
                    for h0 in range(0, 16, 8):
                        st = {}
                        def epi_r(j, pt, pb, h0=h0, st=st):
                            s32, s32b = st32.next()
                            s16, s16b = st16.next()
                            evac_copy(s32[:, 0:512], pt[:, 0:512], [pb], [s32b])
                            if sample:
                                rope(s32[:, 0:512].rearrange("p (h d) -> p h d", d=64), s32b, 8, 64, rm[:, j], rb_, tmp, tmpb)
                            evac_copy(s16[:, 0:512], s32[:, 0:512], [s32b], [s16b])
                            if j == 0:
                                st["t"] = qr_st.next()
                            q_t, q_b = st["t"]
                            transpose_blocks([s16[:, hh * 64:(hh + 1) * 64] for hh in range(8)],
                                             lambda i, n, w: q_t[0:64, i:i + n, j * 128:(j + 1) * 128], [s16b], q_b)
                            if j == 3:
                                k.dma("pool", out=qTr[h0:h0 + 8, :, row0:row0 + TG].rearrange("h d t -> d h t"), in_=q_t[:],
                                      reads=[q_b], writes=[scrb])
                        jobs.append((wq3[:, h0:h0 + 8, 128:192], 512, "A", epi_r))
                    gemm_list(AT, ATb, 8, jobs, wr, pring)
                for g in range(NKG):
                    krow0 = g * TG
                    load_AT(AT, ATb, ckv_s, krow0, 512, a_ring)
                    jobs = []
                    for h0 in range(0, 16, 4):
                        def epi_kn(j, pt, pb, h0=h0):
                            s16, s16b = st16.next()
                            evac_copy(s16[:, 0:TG], pt[:, 0:TG], [pb], [s16b])
                            k.dma("pool", out=kTn[h0 + j, :, krow0:krow0 + TG], in_=s16[:, 0:TG], reads=[s16b], writes=[scrb])
                        jobs.append((wkv3[:, h0:h0 + 4, 0:128], 512, "B", epi_kn))
                    for h0 in range(0, 16, 4):
                        def epi_v(j, pt, pb, h0=h0):
                            s16, s16b = st16.next()
                            evac_copy(s16[:, 0:512], pt[:, 0:512], [pb], [s16b])
                            kt = krow0 // 128 + j
                            k.dma("pool", out=vm[h0:h0 + 4, :, kt, :].rearrange("h p d -> p h d"),
                                  in_=s16[:, 0:512].rearrange("p (h d) -> p h d", d=128), reads=[s16b], writes=[scrb])
                        jobs.append((wkv3[:, h0:h0 + 4, 128:256], 512, "A", epi_v))
                    gemm_list(AT, ATb, 4, jobs, wr, pring)
                k.barrier()

        if stop_after >= 4:
            with ExitStack() as ps:
                MAXK = (TS + TC)
                qn = Ring([sb(ps, "qn%d" % i, [128, TS], BF16) for i in range(2)])
                qr = Ring([sb(ps, "qr%d" % i, [64, TS], BF16) for i in range(2)])
                kn = Ring([sb(ps, "kn%d" % i, [128, MAXK], BF16) for i in range(2)])
                kr_ = Ring([sb(ps, "kr%d" % i, [64, MAXK], BF16) for i in range(2)])
                vv = Ring([sb(ps, "vv%d" % i, [128, MAXK // 128, 130], BF16) for i in range(2)])
                pp = Ring([sb(ps, "pp%d" % i, [128, 512], BF16) for i in range(4)])
                ost = Ring([sb(ps, "ost%d" % i, [128, 128], BF16) for i in range(4)])
                rcp = Ring([sb(ps, "rcp%d" % i, [128, 1], F32) for i in range(4)])
                class SRing:
                    def __init__(self):
                        self.t = [pf[0], pf[1], ptb[0][:].bitcast(F32), ptb[1][:].bitcast(F32)]
                        self.b = [pfb[0], pfb[1], pT.bufs[0], pT.bufs[1]]
                        self.i = 0

                    def next(self):
                        j = self.i
                        self.i = (self.i + 1) % 4
                        return self.t[j], self.b[j]
                sring = SRing()
                for (q0, nq, k0, nk) in seqs:
                    nkt = nk // 128
                    for typ in ("mla", "gqa"):
                        for h in range(16):
                            flush_casts(1)
                            qn_t, qn_b = qn.next()
                            kn_t, kn_b = kn.next()
                            v_t, v_b = vv.next()
                            if typ == "mla":
                                qr_t, qr_b = qr.next()
                                kr_t, kr_b = kr_.next()
                                k.dma("sp", out=qn_t[:, :nq], in_=qTn[h, :, q0:q0 + nq], reads=[scrb], writes=[qn_b])
                                k.dma("sp", out=qr_t[:, :nq], in_=qTr[h, :, q0:q0 + nq], reads=[scrb], writes=[qr_b])
                                k.dma("sp", out=kn_t[:, :nk], in_=kTn[h, :, k0:k0 + nk], reads=[scrb], writes=[kn_b])
                                k.dma("sp", out=kr_t[:, :nk], in_=kTr[:, k0:k0 + nk], reads=[scrb], writes=[kr_b])
                                k.dma("sp", out=v_t[:, :nkt, 0:128], in_=vm[h, :, k0 // 128:k0 // 128 + nkt, :], reads=[scrb], writes=[v_b])
                                scale = 192.0 ** -0.5
                                odst = o_mla
                            else:
                                k.dma("sp", out=qn_t[:, :nq], in_=gqT[h, :, q0:q0 + nq], reads=[scrb], writes=[qn_b])
                                k.dma("sp", out=kn_t[:, :nk], in_=gkT[h // 4, :, k0:k0 + nk], reads=[scrb], writes=[kn_b])
                                k.dma("sp", out=v_t[:, :nkt, 0:128], in_=gvm[h // 4, :, k0 // 128:k0 // 128 + nkt, :], reads=[scrb], writes=[v_b])
                                scale = 128.0 ** -0.5
                                odst = o_gqa
                            k.op("dve", lambda: nc.vector.memset(v_t[:, :nkt, 128:129], 1.0), writes=[v_b])
                            for qg0 in range(0, nq, 512):
                                qw = min(512, nq - qg0)
                                nqs = qw // 128
                                def emit_S(kt):
                                    st_, sb__ = sring.next()
                                    if typ == "mla":
                                        k.op("pe", lambda: nc.tensor.matmul(st_[:, :qw], lhsT=kn_t[:, kt * 128:(kt + 1) * 128],
                                                                            rhs=qn_t[:, qg0:qg0 + qw], start=True, stop=False),
                                             reads=[kn_b, qn_b], writes=[sb__], mark=False)
                                        k.op("pe", lambda: nc.tensor.matmul(st_[:, :qw], lhsT=kr_t[:, kt * 128:(kt + 1) * 128],
                                                                            rhs=qr_t[:, qg0:qg0 + qw], start=False, stop=True),
                                             reads=[kr_b, qr_b], writes=[sb__])
                                    else:
                                        k.op("pe", lambda: nc.tensor.matmul(st_[:, :qw], lhsT=kn_t[:, kt * 128:(kt + 1) * 128],
                                                                            rhs=qn_t[:, qg0:qg0 + qw], start=True, stop=True),
                                             reads=[kn_b, qn_b], writes=[sb__])
                                    return st_, sb__

                                LA = 3
                                pend = [emit_S(kt0) for kt0 in range(min(LA, nkt))]
                                for kt in range(nkt):
                                    st_, sb__ = pend.pop(0)
                                    p_t, p_b = pp.next()
                                    k.op("act", lambda: nc.scalar.activation(out=p_t[:, :qw], in_=st_[:, :qw], func=AF.Exp, scale=scale),
                                         reads=[sb__], writes=[p_b])
                                    if kt + LA < nkt:
                                        pend.append(emit_S(kt + LA))
                                    for qs in range(nqs):
                                        k.op("pe", lambda: nc.tensor.matmul(pf[2 + qs][:, 0:129], lhsT=p_t[:, qs * 128:(qs + 1) * 128],
                                                                            rhs=v_t[:, kt, 0:129], start=(kt == 0), stop=(kt == nkt - 1)),
                                             reads=[p_b, v_b], writes=[pfb[2 + qs]], mark=(kt == nkt - 1 or qs == nqs - 1))
                                for qs in range(nqs):
                                    r_t, r_b = rcp.next()
                                    o_t, o_b = ost.next()
                                    k.op("dve", lambda: nc.vector.reciprocal(out=r_t[:], in_=pf[2 + qs][:, 128:129]),
                                         reads=[pfb[2 + qs]], writes=[r_b])
                                    k.op("dve", lambda: nc.vector.tensor_scalar(out=o_t[:], in0=pf[2 + qs][:, 0:128], scalar1=r_t[:, 0:1],
                                                                                scalar2=None, op0=ALU.mult),
                                         reads=[pfb[2 + qs], r_b], writes=[o_b])
                                    r0 = q0 + qg0 + qs * 128
                                    k.dma("pool", out=odst[r0:r0 + 128, h * 128:(h + 1) * 128], in_=o_t[:], reads=[o_b], writes=[scrb])
                k.barrier()

        flush_casts(1000)
        if stop_after >= 5:
            with ExitStack() as ps:
                ATh = sb(ps, "ATh", [128, 32, TG], BF16)
                AThb = Buf(multi=True)
                ATm = sb(ps, "ATm", [128, 16, TG], BF16)
                ATmb = Buf(multi=True)
                ATg = sb(ps, "ATg", [128, 16, TG], BF16)
                ATgb = Buf(multi=True)
                xt_ring = Ring([sb(ps, "xtc", [128, D], F32)])
                xn = sb(ps, "xnc", [128, D], BF16)
                xnb = Buf()
                small = Ring([sb(ps, "smc%d" % i, [128, 8], F32) for i in range(6)])
                a_ring = Ring([sb(ps, "ac_%d" % i, [128, 2048], BF16) for i in range(2)])
                wr = Ring([sb(ps, "wrc%d" % i, [128, 32, 512], BF16) for i in range(2)])
                sg = sb(ps, "sg", [128, 4, 512], F32)
                sgb = [Buf() for _ in range(4)]
                acc = sb(ps, "accc", [128, 4, 512], F32)
                accb = [Buf() for _ in range(4)]
                tt = Ring([sb(ps, "ttc%d" % i, [128, 512], F32) for i in range(2)])
                o16 = Ring([sb(ps, "o16c%d" % i, [128, 512], BF16) for i in range(3)])
                pring = PRing([0, 1, 2, 3])
                mergedb = Buf(multi=True)
                for g in range(NTG):
                    row0 = g * TG
                    load_hT(ps, ATh, AThb, xin, row0, 0, (xt_ring, xn, xnb, small))
                    load_AT(ATm, ATmb, o_mla, row0, 2048, a_ring)
                    load_AT(ATg, ATgb, o_gqa, row0, 2048, a_ring)
                    jobs = []
                    for cb in range(8):
                        c0 = cb * 512

                        def epi_g(j, pt, pb):
                            k.op("act", lambda: nc.scalar.activation(out=sg[:, j, :], in_=pt[:, 0:512], func=AF.Sigmoid),
                                 reads=[pb], writes=[sgb[j]])

                        def epi_m1(j, pt, pb):
                            k.op("dve", lambda: nc.vector.tensor_tensor(out=acc[:, j, :], in0=pt[:, 0:512], in1=sg[:, j, :], op=ALU.mult),
                                 reads=[pb, sgb[j]], writes=[accb[j]])

                        def epi_m2(j, pt, pb, c0=c0):
                            t_t, t_b = tt.next()
                            o_t, o_b = o16.next()
                            k.op("dve", lambda: nc.vector.tensor_tensor(out=t_t[:], in0=pt[:, 0:512], in1=sg[:, j, :], op=ALU.mult),
                                 reads=[pb, sgb[j]], writes=[t_b])
                            k.op("dve", lambda: nc.vector.tensor_tensor(out=o_t[:], in0=t_t[:], in1=acc[:, j, :], op=ALU.add),
                                 reads=[t_b, accb[j]], writes=[o_b])
                            tok0 = row0 + j * 128
                            k.dma("pool", out=merged[tok0:tok0 + 128, c0:c0 + 512], in_=o_t[:], reads=[o_b], writes=[mergedb])

                        jobs.append((w_in_b[:, 4672 + c0:4672 + c0 + 512], 512, "A", epi_g, ATh, AThb, 32))
                        jobs.append((w_mla_ob[:, c0:c0 + 512], 512, "A", epi_m1, ATm, ATmb, 16))
                        jobs.append((w_in_b[:, 8768 + c0:8768 + c0 + 512], 512, "A", epi_g, ATh, AThb, 32))
                        jobs.append((w_gqa_ob[:, c0:c0 + 512], 512, "A", epi_m2, ATg, ATgb, 16))
                    gemm_list(None, None, None, jobs, wr, pring)
                k.barrier()
            with ExitStack() as ps:
                AT = sb(ps, "ATc2", [128, 32, TG], BF16)
                ATb = Buf(multi=True)
                a_ring = Ring([sb(ps, "ac2_%d" % i, [128, D], BF16) for i in range(2)])
                wr = Ring([sb(ps, "wrc2%d" % i, [128, 32, 512], BF16) for i in range(2)])
                g1bc = sb(ps, "g1bc", [128, D], F32)
                g1b = Buf()
                xs = Ring([sb(ps, "xs%d" % i, [128, 512], F32) for i in range(3)])
                tt = Ring([sb(ps, "ttd%d" % i, [128, 512], F32) for i in range(3)])
                pring = PRing([0, 1, 2, 3])
                x1b = Buf(multi=True)
                cur_set = None
                for g in range(NTG):
                    row0 = g * TG
                    sset = 0 if row0 < TS else 1
                    if sset != cur_set:
                        k.dma("sp", out=g1bc[:], in_=mods[sset, 2 * D:3 * D].partition_broadcast(128), reads=[modb], writes=[g1b])
                        cur_set = sset
                    load_AT(AT, ATb, merged, row0, D, a_ring)
                    jobs = []
                    for cb in range(8):
                        c0 = cb * 512

                        def epi_o(j, pt, pb, c0=c0):
                            tok0 = row0 + j * 128
                            x_t, x_b = xs.next()
                            t_t, t_b = tt.next()
                            k.dma("sp", out=x_t[:], in_=xin[tok0:tok0 + 128, c0:c0 + 512], writes=[x_b])
                            k.op("dve", lambda: nc.vector.tensor_tensor(out=t_t[:], in0=pt[:, 0:512], in1=g1bc[:, c0:c0 + 512], op=ALU.mult),
                                 reads=[pb, g1b], writes=[t_b])
                            k.op("dve", lambda: nc.vector.tensor_tensor(out=t_t[:], in0=t_t[:], in1=x_t[:], op=ALU.add),
                                 reads=[t_b, x_b], writes=[t_b])
                            k.dma("pool", out=x1[tok0:tok0 + 128, c0:c0 + 512], in_=t_t[:], reads=[t_b], writes=[x1b])

                        jobs.append((w_out_b[:, c0:c0 + 512], 512, "A", epi_o))
                    gemm_list(AT, ATb, 32, jobs, wr, pring)
                k.barrier()

        if stop_after >= 6 or only == "d1b":
            with ExitStack() as ps:
                AT = sb(ps, "ATd", [128, 32, TG], BF16)
                ATb = Buf(multi=True)
                xt_ring = Ring([sb(ps, "xtd", [128, D], F32)])
                xn = sb(ps, "xnd", [128, D], BF16)
                xnb = Buf()
                small = Ring([sb(ps, "smd%d" % i, [128, 8], F32) for i in range(6)])
                wr = Ring([sb(ps, "wrd%d" % i, [128, 32, 512], BF16) for i in range(2)])
                st16 = Ring([sb(ps, "s16d_%d" % i, [128, 512], BF16) for i in range(3)])
                pring = PRing([0, 1, 2, 3])
                qpb = Buf(multi=True)
                for g in range(0 if only else NTG):
                    row0 = g * TG
                    load_hT(ps, AT, ATb, x1, row0, 1, (xt_ring, xn, xnb, small))
                    jobs = []
                    for c in range(4):
                        def epi_q(j, pt, pb, c=c):
                            s16, s16b = st16.next()
                            evac_copy(s16[:, 0:TG], pt[:, 0:TG], [pb], [s16b])
                            k.dma("pool", out=qpT[c * 4 + j, :, row0:row0 + TG], in_=s16[:, 0:TG], reads=[s16b], writes=[qpb])
                        jobs.append((w_pq_b[:, c * 512:(c + 1) * 512], 512, "B", epi_q))
                    gemm_list(AT, ATb, 32, jobs, wr, pring)
                k.barrier()

            with ExitStack() as ps:
                V = nc.vector
                skf = sb(ps, "skf", [128, 2, 128], F32)
                skT = sb(ps, "skT", [128, 2, 128], BF16)
                skb = Buf()
                qt_ring = Ring([sb(ps, "qt%d" % i, [128, 16, 128], BF16) for i in range(2)])
                S = sb(ps, "S", [128, 16, 128], F32)
                Sb = Buf()
                S2 = sb(ps, "S2", [128, 16, 128], F32)
                S2b = Buf()
                sv = sb(ps, "sv", [128, 16, 16], F32)
                si_u = sb(ps, "si_u", [128, 16, 16], U32)
                si_f = sb(ps, "si_f", [128, 16, 16], F32)
                svb = Buf()
                cand = sb(ps, "cand", [128, 8, 256], F32)
                cand2 = S2[:].rearrange("p a m -> p (a m)").rearrange("p (h c) -> p h c", c=256)
                candb = Buf()
                bv = sb(ps, "bv", [128, 8, 16], F32)
                bp_u = sb(ps, "bp_u", [128, 8, 16], U32)
                il_u = sb(ps, "il_u", [128, 8, 16], U32)
                jl_u = sb(ps, "jl_u", [128, 8, 16], U32)
                il_f = sb(ps, "il_f", [128, 8, 16], F32)
                jl_f = sb(ps, "jl_f", [128, 8, 16], F32)
                bvb = Buf()
                oh = S[:].rearrange("p a m -> p (a m)").rearrange("p (h a l) -> p h a l", h=8, a=16)
                ohb = Sb
                sel = sb(ps, "sel", [128, 3, 128], F32)
                selb = Buf()
                zz = sb(ps, "zz", [128, 8], F32)
                T3 = sb(ps, "T3", [128, 3, 128], F32)
                T3b = Buf()
                Cring = Ring([sb(ps, "Cm%d" % i, [128, 128, 128], BF16) for i in range(2)])
                Rring = Ring([sb(ps, "Rm%d" % i, [128, 128, 128], BF16) for i in range(2)])
                Gst = Ring([sb(ps, "Gst%d" % i, [128, 128, 128], BF16) for i in range(1)], multi=True)
                Gsb = Buf(multi=True)
                gring = PRing([4, 5])
                k.dma("sp", out=skf[:], in_=sub_keys.rearrange("p m k -> m p k"), writes=[skb])
                ptk, pbk = pf[0], pfb[0]
                for p_ in range(2):
                    k.op("pe", lambda: nc.tensor.transpose(ptk[:, p_ * 128:(p_ + 1) * 128], skf[:, p_, :], ident_f[:]),
                         reads=[skb, cb_], writes=[pbk], mark=(p_ == 1))
                k.op("dve", lambda: V.tensor_copy(out=skT[:].rearrange("p a m -> p (a m)"), in_=ptk[:, 0:256]), reads=[pbk], writes=[skb])
                iota16 = iota_f[:, 0:16].unsqueeze(1).unsqueeze(1).broadcast_to([128, 8, 16, 16])
                def emit_scores(nt):
                    qt, qb = qt_ring.next()
                    k.dma("sp", out=qt[:], in_=qpT[:, :, nt * 128:(nt + 1) * 128].rearrange("a k t -> k a t"), reads=[qpb], writes=[qb])
                    for pair in range(16):
                        bank = pair // 4
                        k.op("pe", lambda: nc.tensor.matmul(pf[bank][:, (pair % 4) * 128:(pair % 4 + 1) * 128], lhsT=qt[:, pair, :],
                                                            rhs=skT[:, pair % 2, :], start=True, stop=True),
                             reads=[qb, skb], writes=[pfb[bank]], mark=(pair % 4 == 3))
                    for bank in range(4):
                        k.op("act", lambda: nc.scalar.copy(out=S[:, bank * 4:(bank + 1) * 4, :],
                                                           in_=pf[bank][:, 0:512].rearrange("p (a m) -> p a m", m=128)),
                             reads=[pfb[bank]], writes=[Sb])

                n_tiles = cfg.get('d1b_nt', NQT)
                if n_tiles > 0:
                    emit_scores(0)
                for nt in range(n_tiles):
                    for pair in range(16):
                        k.op("dve", lambda: V.max(out=sv[:, pair, 0:8], in_=S[:, pair, :]), reads=[Sb], writes=[svb])
                        k.op("dve", lambda: V.max_index(out=si_u[:, pair, 0:8], in_max=sv[:, pair, 0:8], in_values=S[:, pair, :]),
                             reads=[Sb, svb], writes=[svb])
                        k.op("dve", lambda: V.match_replace(out=S2[:, pair, :], in_to_replace=sv[:, pair, 0:8], in_values=S[:, pair, :],
                                                            imm_value=NEG), reads=[Sb, svb], writes=[S2b])
                        k.op("dve", lambda: V.max(out=sv[:, pair, 8:16], in_=S2[:, pair, :]), reads=[S2b], writes=[svb])
                        k.op("dve", lambda: V.max_index(out=si_u[:, pair, 8:16], in_max=sv[:, pair, 8:16], in_values=S2[:, pair, :]),
                             reads=[S2b, svb], writes=[svb])
                    k.op("dve", lambda: V.tensor_copy(out=si_f[:], in_=si_u[:]), reads=[svb], writes=[svb])
                    sv4 = sv[:].rearrange("p (h a) t -> p h a t", a=2)
                    si4 = si_f[:].rearrange("p (h a) t -> p h a t", a=2)
                    c4 = cand[:].rearrange("p h (i j) -> p h i j", j=16)
                    k.op("dve", lambda: V.tensor_tensor(out=c4, in0=sv4[:, :, 0, :].unsqueeze(3).broadcast_to([128, 8, 16, 16]),
                                                        in1=sv4[:, :, 1, :].unsqueeze(2).broadcast_to([128, 8, 16, 16]), op=ALU.add),
                         reads=[svb], writes=[candb])
                    for h in range(8):
                        k.op("dve", lambda: V.max(out=bv[:, h, 0:8], in_=cand[:, h, :]), reads=[candb], writes=[bvb])
                        k.op("dve", lambda: V.max_index(out=bp_u[:, h, 0:8], in_max=bv[:, h, 0:8], in_values=cand[:, h, :]),
                             reads=[candb, bvb], writes=[bvb])
                        k.op("dve", lambda: V.match_replace(out=cand2[:, h, :], in_to_replace=bv[:, h, 0:8], in_values=cand[:, h, :],
                                                            imm_value=NEG), reads=[candb, bvb], writes=[S2b])
                        k.op("dve", lambda: V.max(out=bv[:, h, 8:16], in_=cand2[:, h, :]), reads=[S2b], writes=[bvb])
                        k.op("dve", lambda: V.max_index(out=bp_u[:, h, 8:16], in_max=bv[:, h, 8:16], in_values=cand2[:, h, :]),
                             reads=[S2b, bvb], writes=[bvb])
                    k.op("dve", lambda: V.tensor_single_scalar(out=il_u[:], in_=bp_u[:], scalar=4, op=ALU.logical_shift_right),
                         reads=[bvb], writes=[bvb])
                    k.op("dve", lambda: V.tensor_single_scalar(out=jl_u[:], in_=bp_u[:], scalar=15, op=ALU.bitwise_and),
                         reads=[bvb], writes=[bvb])
                    k.op("dve", lambda: V.tensor_copy(out=il_f[:], in_=il_u[:]), reads=[bvb], writes=[bvb])
                    k.op("dve", lambda: V.tensor_copy(out=jl_f[:], in_=jl_u[:]), reads=[bvb], writes=[bvb])
                    for a_, (lf, dsti) in enumerate(((il_f, 0), (jl_f, 1))):
                        k.op("dve", lambda: V.tensor_tensor(out=oh[:], in0=lf[:].unsqueeze(3).broadcast_to([128, 8, 16, 16]), in1=iota16,
                                                            op=ALU.is_equal), reads=[bvb, cb_], writes=[ohb])
                        k.op("dve", lambda: V.tensor_tensor(out=oh[:], in0=oh[:], in1=si4[:, :, a_, :].unsqueeze(2).broadcast_to([128, 8, 16, 16]),
                                                            op=ALU.mult), reads=[ohb, svb], writes=[ohb])
                        k.op("dve", lambda: V.tensor_reduce(out=sel[:, dsti, :].rearrange("p (h a) -> p h a", a=16), in_=oh[:], axis=AX.X, op=ALU.add),
                             reads=[ohb], writes=[selb])
                    g3 = sel[:, 2, :].rearrange("p (h a) -> p h a", a=16)
                    k.op("dve", lambda: V.tensor_tensor(out=g3, in0=bv[:], in1=bv[:, :, 0:1].broadcast_to([128, 8, 16]), op=ALU.subtract),
                         reads=[bvb], writes=[selb])
                    k.op("act", lambda: nc.scalar.activation(out=g3, in_=g3, func=AF.Exp), reads=[selb], writes=[selb])
                    k.op("dve", lambda: V.tensor_reduce(out=zz[:], in_=g3, axis=AX.X, op=ALU.add), reads=[selb], writes=[ohb])
                    k.op("dve", lambda: V.reciprocal(out=zz[:], in_=zz[:]), reads=[ohb], writes=[ohb])
                    k.op("dve", lambda: V.tensor_tensor(out=g3, in0=g3, in1=zz[:].unsqueeze(2).broadcast_to([128, 8, 16]), op=ALU.mult),
                         reads=[selb, ohb], writes=[selb])
                    ptt, pbt = gring.next()
                    for a_ in range(3):
                        k.op("pe", lambda: nc.tensor.transpose(ptt[:, a_ * 128:(a_ + 1) * 128], sel[:, a_, :], ident_f[:]),
                             reads=[selb, cb_], writes=[pbt], mark=(a_ == 2))
                    k.op("dve", lambda: V.tensor_copy(out=T3[:].rearrange("p a t -> p (a t)"), in_=ptt[:, 0:384]), reads=[pbt], writes=[T3b])
                    iob = iota_f[:].unsqueeze(1).broadcast_to([128, 128, 128])
                    Cm, Cb = Cring.next()
                    Rm, Rb = Rring.next()
                    k.op("dve", lambda: V.tensor_tensor(out=Cm[:], in0=iob, in1=T3[:, 1, :].unsqueeze(2).broadcast_to([128, 128, 128]),
                                                        op=ALU.is_equal), reads=[T3b, cb_], writes=[Cb])
                    k.op("dve", lambda: V.tensor_tensor(out=Rm[:], in0=iob, in1=T3[:, 0, :].unsqueeze(2).broadcast_to([128, 128, 128]),
                                                        op=ALU.is_equal), reads=[T3b, cb_], writes=[Rb])
                    k.op("pool", lambda: nc.gpsimd.tensor_tensor(out=Rm[:], in0=Rm[:], in1=T3[:, 2, :].unsqueeze(2).broadcast_to([128, 128, 128]),
                                                                 op=ALU.mult), reads=[T3b, Rb], writes=[Rb])
                    if nt + 1 < n_tiles:
                        emit_scores(nt + 1)
                    gst, gstb = Gst.next()
                    for n0 in range(0, 128, 4):
                        ptg, pbg = gring.next()
                        for t in range(4):
                            n = n0 + t
                            k.op("pe", lambda: nc.tensor.matmul(ptg[:, t * 128:(t + 1) * 128], lhsT=Cm[:, n, :], rhs=Rm[:, n, :],
                                                                start=True, stop=True), reads=[Cb, Rb], writes=[pbg], mark=(t == 3))
                        k.op("act", lambda: nc.scalar.copy(out=gst[:, :, n0:n0 + 4], in_=ptg[:, 0:512].rearrange("p (n i) -> p i n", i=128)),
                             reads=[pbg], writes=[gstb])
                    k.dma("pool", out=Gs[nt], in_=gst[:], reads=[gstb], writes=[Gsb])
                k.barrier()

        if stop_after >= 7:
            with ExitStack() as ps:
                uring = Ring([sb(ps, "ur%d" % i, [128, D], F32) for i in range(2)])
                ustr = Ring([sb(ps, "us%d" % i, [128, 32, 128], BF16) for i in range(2)], multi=True)
                pring = PRing([0, 1, 2, 3])
                utb = Buf(multi=True)
                for eb in range(128):
                    ut, ub = uring.next()
                    k.dma("sp", out=ut[:], in_=peer_u[eb * 128:(eb + 1) * 128, :], writes=[ub])
                    us, usb = ustr.next()
                    for g4 in range(8):
                        pt, pb = pring.next()
                        for t in range(4):
                            dc = g4 * 4 + t
                            k.op("pe", lambda: nc.tensor.transpose(pt[:, t * 128:(t + 1) * 128], ut[:, dc * 128:(dc + 1) * 128], ident_f[:]),
                                 reads=[ub, cb_], writes=[pb], mark=(t == 3))
                        evac_copy(us[:, g4 * 4:(g4 + 1) * 4, :], pt[:, 0:512].rearrange("p (t e) -> p t e", e=128), [pb], [usb])
                    k.dma("pool", out=ut_b[eb], in_=us[:], reads=[usb], writes=[utb])
                k.barrier()
            with ExitStack() as ps:
                V = nc.vector
                AT = sb(ps, "ATe", [128, 32, TG], BF16)
                ATb = Buf(multi=True)
                xt_ring = Ring([sb(ps, "xte", [128, D], F32)])
                xn = sb(ps, "xne", [128, D], BF16)
                xnb = Buf()
                small = Ring([sb(ps, "sme%d" % i, [128, 8], F32) for i in range(6)])
                acc = sb(ps, "acce", [128, 4, D], F32)
                accb = [Buf() for _ in range(4)]
                utr = Ring([sb(ps, "utr%d" % i, [128, 32, 128], BF16) for i in range(2)])
                vr = Ring([sb(ps, "vr%d" % i, [128, D], BF16) for i in range(5)])
                wT = Ring([sb(ps, "wT%d" % i, [128, 4, TG], BF16) for i in range(2)], multi=True)
                ge = Ring([sb(ps, "ge%d" % i, [128, TG], F32) for i in range(2)])
                gt = Ring([sb(ps, "gt%d" % i, [128, 4, 128], BF16) for i in range(3)])
                gch = Ring([sb(ps, "gch%d" % i, [128, 1024], F32) for i in range(2)])
                aring = PRing([0, 1])
                oring = PRing([2, 3, 4, 5])
                EG = 4
                for g in range(NTG):
                    row0 = g * TG
                    sset = 0 if row0 < TS else 1
                    load_hT(ps, AT, ATb, x1, row0, 1, (xt_ring, xn, xnb, small))
                    for eg in range(128 // EG):
                        w_t, w_b = wT.next()
                        vts = []
                        for e_ in range(EG):
                            i = eg * EG + e_
                            u_t, u_b = utr.next()
                            k.dma("sp", out=u_t[:], in_=ut_b[i], reads=[utb], writes=[u_b])
                            g_t, g_b = gt.next()
                            k.dma("sp", out=g_t[:], in_=Gs[g * 4:(g + 1) * 4, :, i, :].rearrange("t j n -> j t n"), reads=[Gsb], writes=[g_b])
                            v_t, v_b = vr.next()
                            k.dma("sp", out=v_t[:], in_=pv_b[i * 128:(i + 1) * 128, :], reads=[wb_of.get("pv_b", wbuf0)], writes=[v_b])
                            vts.append((v_t, v_b))
                            pa, pab = aring.next()
                            for dc in range(32):
                                k.op("pe", lambda: nc.tensor.matmul(pa[:, 0:TG], lhsT=u_t[:, dc, :], rhs=AT[:, dc, :], start=(dc == 0), stop=(dc == 31)),
                                     reads=[u_b, ATb], writes=[pab], mark=(dc == 31))
                            ge_t, ge_b = ge.next()
                            k.op("act", lambda: nc.scalar.activation(out=ge_t[:], in_=pa[:, 0:TG], func=AF.Gelu), reads=[pab], writes=[ge_b])
                            k.op("dve", lambda: V.tensor_tensor(out=w_t[:, e_, :], in0=ge_t[:], in1=g_t[:].rearrange("p t n -> p (t n)"), op=ALU.mult),
                                 reads=[ge_b, g_b], writes=[w_b])
                        for js in range(4):
                            for db in range(8):
                                po, pob = oring.next()
                                for e_ in range(EG):
                                    k.op("pe", lambda: nc.tensor.matmul(po[:, 0:512], lhsT=w_t[:, e_, js * 128:(js + 1) * 128],
                                                                        rhs=vts[e_][0][:, db * 512:(db + 1) * 512], start=(e_ == 0), stop=(e_ == EG - 1)),
                                         reads=[w_b, vts[e_][1]], writes=[pob], mark=(e_ == EG - 1))
                                dst = acc[:, js, db * 512:(db + 1) * 512]
                                if eg == 0:
                                    k.op("dve", lambda: V.tensor_copy(out=dst, in_=po[:, 0:512]), reads=[pob], writes=[accb[js]])
                                else:
                                    k.op("dve", lambda: V.tensor_tensor(out=dst, in0=po[:, 0:512], in1=dst, op=ALU.add), reads=[pob, accb[js]], writes=[accb[js]])
                    for js in range(4):
                        tok0 = row0 + js * 128
                        xt, xb = xt_ring.next()
                        k.dma("sp", out=xt[:], in_=x1[tok0:tok0 + 128, :], writes=[xb])
                        for cc in range(4):
                            gc, gcb = gch.next()
                            k.dma("sp", out=gc[:], in_=mods[sset, 5 * D + cc * 1024:5 * D + (cc + 1) * 1024].partition_broadcast(128),
                                  reads=[modb], writes=[gcb])
                            a_ = acc[:, js, cc * 1024:(cc + 1) * 1024]
                            k.op("dve", lambda: V.tensor_tensor(out=a_, in0=a_, in1=gc[:], op=ALU.mult), reads=[accb[js], gcb], writes=[accb[js]])
                            k.op("dve", lambda: V.tensor_tensor(out=a_, in0=a_, in1=xt[:, cc * 1024:(cc + 1) * 1024], op=ALU.add),
                                 reads=[accb[js], xb], writes=[accb[js]])
                        sst, sb_ = small.next()
                        k.op("act", lambda: nc.scalar.activation(out=xn[:], in_=acc[:, js, :], func=AF.Square, accum_out=sst[:, 0:1]),
                             reads=[accb[js]], writes=[xnb, sb_])
                        ssb[0] = sb_
                        rs, rb = rstd_of(sst[:, 0:1], 1, D, small)
                        for cc in range(4):
                            gc, gcb = gch.next()
                            k.dma("sp", out=gc[:], in_=g_final[cc * 1024:(cc + 1) * 1024].partition_broadcast(128), writes=[gcb])
                            a_ = acc[:, js, cc * 1024:(cc + 1) * 1024]
                            k.op("dve", lambda: V.scalar_tensor_tensor(out=a_, in0=a_, scalar=rs[:, 0:1], in1=gc[:], op0=ALU.mult, op1=ALU.mult),
                                 reads=[accb[js], rb, gcb], writes=[accb[js]])
                        k.dma("pool", out=y[tok0:tok0 + 128, :], in_=acc[:, js, :], reads=[accb[js]])
                k.barrier()

        k.barrier()
    return nc


def _host_rope(TS):
    def tab(dim):
        n_rows = TS // 64
        rows = np.broadcast_to(np.arange(n_rows)[:, None], (n_rows, 64)).reshape(-1).astype(np.float32)
        cols = np.broadcast_to(np.arange(64)[None, :], (n_rows, 64)).reshape(-1).astype(np.float32)
        n_freq = dim // 4
        freqs = (np.float32(10000.0) ** (-np.arange(n_freq, dtype=np.float32) / np.float32(n_freq))).astype(np.float32)
        ang = np.concatenate([rows[:, None] * freqs, cols[:, None] * freqs], axis=-1).astype(np.float32)
        return np.stack([np.cos(ang), np.sin(ang)], axis=1).astype(np.float32)
    return tab(64), tab(128)


def make_in_maps(inp, cfg, n_cores):
    TS, TC, TP, NPB = cfg["TS"], cfg["TC"], cfg["TP"], cfg["NPB"]
    rm, rg = _host_rope(TS)
    f = lambda a: np.ascontiguousarray(np.asarray(a, dtype=np.float32))
    shared = {
        "w_mod": f(inp["w_mod"][0]), "b_mod": f(inp["b_mod"][0]), "g_norm1": f(inp["g_norm1"][0]),
        "g_norm2": f(inp["g_norm2"][0]), "w_in": f(inp["w_in"][0]), "g_q_a": f(inp["g_q_a"][0]),
        "w_q_b": f(inp["w_q_b"][0]), "g_kv_a": f(inp["g_kv_a"][0]), "w_kv_b": f(inp["w_kv_b"][0]),
        "g_gqa_q": f(inp["g_gqa_q"][0]), "g_gqa_k": f(inp["g_gqa_k"][0]), "w_mla_o": f(inp["w_mla_o"][0]),
        "w_gqa_o": f(inp["w_gqa_o"][0]), "w_out": f(inp["w_out"][0]), "w_peer_q": f(inp["w_peer_q"][0]),
        "sub_keys": f(inp["peer_sub_keys"][0]), "peer_u": f(inp["peer_u"][0]), "peer_v": f(inp["peer_v"][0]),
        "g_final": f(inp["g_final"]), "rope_m": rm, "rope_g": rg,
    }
    maps = []
    for b in range(n_cores):
        m = dict(shared)
        xp = np.asarray(inp["x_prompt"][NPB * b:NPB * (b + 1)], dtype=np.float32).reshape(NPB * TP, D)
        m["xin"] = np.ascontiguousarray(np.concatenate([np.asarray(inp["x_sample"][b], dtype=np.float32), xp], axis=0))
        m["cvec"] = np.ascontiguousarray(np.stack([np.asarray(inp["c"][b], dtype=np.float32),
                                                   np.asarray(inp["c_ctx"], dtype=np.float32)], axis=0))
        m["c_ckv"] = f(inp["cache_mla_ckv"][b, 0])
        m["c_kr"] = f(inp["cache_mla_krope"][b, 0])
        m["c_gk"] = f(inp["cache_gqa_k"][b, 0]).reshape(TC, 512)
        m["c_gv"] = f(inp["cache_gqa_v"][b, 0]).reshape(TC, 512)
        maps.append(m)
    return maps


FULL_CFG = dict(TS=4096, TC=512, TP=256, NPB=2)


def kernel(**inp):
    cfg = FULL_CFG
    n = 8
    nc = build(cfg)
    maps = make_in_maps(inp, cfg, n)
    res = run_bass_kernel_spmd(nc, maps, core_ids=list(range(n)))
    TS, TP, NPB = cfg["TS"], cfg["TP"], cfg["NPB"]
    R = res.results
    y_s = np.stack([R[b]["y"][:TS] for b in range(n)], axis=0)
    y_p = np.concatenate([R[b]["y"][TS:].reshape(NPB, TP, D) for b in range(n)], axis=0)
    ckv = np.concatenate([R[b]["n_ckv"].reshape(NPB, 1, TP, 512) for b in range(n)], axis=0)
    kr = np.concatenate([R[b]["n_kr"].reshape(NPB, 1, TP, 64) for b in range(n)], axis=0)
    gk = np.concatenate([R[b]["n_gk"].reshape(NPB, 1, TP, 4, 128) for b in range(n)], axis=0)
    gv = np.concatenate([R[b]["n_gv"].reshape(NPB, 1, TP, 4, 128) for b in range(n)], axis=0)
    return (y_p.astype(np.float32), y_s.astype(np.float32), ckv.astype(np.float32), kr.astype(np.float32),
            gk.astype(np.float32), gv.astype(np.float32))
```
